# Optimizing a Trainium2 kernel written in Bass

```python
import math
import jax
import jax.numpy as jnp
from jax import lax
import numpy as np

D_MODEL = 1024
BATCH = 16
SEQ = 2048
DEPTH = 1

D_MIX = D_MODEL
DA_HEADS = 4
DA_HEAD_DIM = D_MIX // 16
DA_V_DIM = 2 * DA_HEAD_DIM
DA_WIDTH = DA_HEADS * DA_V_DIM
ML_HEADS = 4
ML_WIDTH = D_MIX - DA_WIDTH
ML_HEAD_DIM = ML_WIDTH // ML_HEADS
ML_CHUNK = 128
CONV_K = 4
ROPE_THETA = 10000.0
Q_BLOCK = 128
RMS_EPS = 1e-6
N_GROUPS = 4
EXPERTS_PER_GROUP = 8
N_EXPERTS = N_GROUPS * EXPERTS_PER_GROUP
TOP_K = 2
D_EXPERT = D_MODEL // 2
MOE_BLOCK = 128
COL_WIDTHS = (DA_HEADS * 2 * DA_HEAD_DIM, DA_HEADS * 2 * DA_HEAD_DIM, DA_WIDTH,
              ML_WIDTH, ML_WIDTH, ML_WIDTH, ML_WIDTH, ML_HEADS, ML_HEADS)
D_IN = sum(COL_WIDTHS)

kernel_name = 'hybrid_diffattn_mlstm_hmoe_layer'


def rmsnorm(x, g):
    xf = x.astype(jnp.float32)
    y = xf * lax.rsqrt(jnp.mean(xf * xf, axis=-1, keepdims=True) + RMS_EPS)
    return (y * g.astype(jnp.float32)).astype(x.dtype)


def split_cols(t):
    out, off = [], 0
    for w in COL_WIDTHS:
        out.append(t[..., off:off + w])
        off += w
    return out


def rope_tables(seq, dim):
    inv = 1.0 / (ROPE_THETA ** (jnp.arange(0, dim, 2, dtype=jnp.float32) / dim))
    ang = jnp.arange(seq, dtype=jnp.float32)[:, None] * inv[None, :]
    return jnp.cos(ang), jnp.sin(ang)


def apply_rope(x, cos, sin):
    x1, x2 = jnp.split(x, 2, axis=-1)
    c = cos.astype(x.dtype)
    s = sin.astype(x.dtype)
    return jnp.concatenate([x1 * c - x2 * s, x1 * s + x2 * c], axis=-1)


def diff_attention(q, k, v, lam):
    B, H, _, S, dh = q.shape
    nb = S // Q_BLOCK
    scale = dh ** -0.5
    qb = q.reshape(B, H, 2, nb, Q_BLOCK, dh).transpose(3, 0, 1, 2, 4, 5)
    key_pos = jnp.arange(S)

    def block(args):
        qi, bi = args
        s = jnp.einsum('bhcqd,bhckd->bhcqk', qi, k).astype(jnp.float32) * scale
        q_pos = bi * Q_BLOCK + jnp.arange(Q_BLOCK)
        mask = key_pos[None, :] <= q_pos[:, None]
        p = jax.nn.softmax(jnp.where(mask, s, -jnp.inf), axis=-1)
        a = p[:, :, 0] - lam * p[:, :, 1]
        return jnp.einsum('bhqk,bhkd->bhqd', a.astype(v.dtype), v)

    o = lax.map(block, (qb, jnp.arange(nb)))
    return o.transpose(1, 2, 0, 3, 4).reshape(B, H, S, v.shape[-1])


def causal_conv(x, w, b):
    S = x.shape[1]
    xp = jnp.pad(x, ((0, 0), (CONV_K - 1, 0), (0, 0)))
    y = b
    for j in range(CONV_K):
        y = y + xp[:, j:j + S] * w[j]
    return y


def mlstm_chunkwise(q, k, v, i_pre, f_pre):
    dtype = q.dtype
    f32 = jnp.float32
    B, H, S, d = q.shape
    L = ML_CHUNK
    nc = S // L
    q = q.astype(f32)
    k = k.astype(f32) * (d ** -0.5)
    v = v.astype(f32)
    log_f = jax.nn.log_sigmoid(f_pre.astype(f32))
    log_i = i_pre.astype(f32)

    def to_chunks(t):
        return jnp.moveaxis(t.reshape(B, H, nc, L, *t.shape[3:]), 2, 0)

    xs = (to_chunks(q), to_chunks(k), to_chunks(v), to_chunks(log_f), to_chunks(log_i))
    causal = jnp.tril(jnp.ones((L, L), dtype=bool))

    def step(carry, inp):
        C, n, m = carry
        qj, kj, vj, lf, li = inp
        b = jnp.cumsum(lf, axis=-1)
        Dm = jnp.where(causal, b[..., :, None] - b[..., None, :] + li[..., None, :], -jnp.inf)
        inter = b + m[..., None]
        m_row = jnp.maximum(inter, jnp.max(Dm, axis=-1))
        Sm = jnp.exp(Dm - m_row[..., None]) * jnp.einsum('bhld,bhsd->bhls', qj, kj)
        sc_in = jnp.exp(inter - m_row)
        num = jnp.einsum('bhls,bhsd->bhld', Sm, vj) + sc_in[..., None] * jnp.einsum('bhvk,bhlk->bhlv', C, qj)
        den = jnp.sum(Sm, axis=-1) + sc_in * jnp.einsum('bhk,bhlk->bhl', n, qj)
        h = num / jnp.maximum(jnp.abs(den), jnp.exp(-m_row))[..., None]
        bL = b[..., -1]
        dec = bL[..., None] - b + li
        m_new = jnp.maximum(bL + m, jnp.max(dec, axis=-1))
        ws = jnp.exp(dec - m_new[..., None])
        sc = jnp.exp(bL + m - m_new)
        C_new = sc[..., None, None] * C + jnp.einsum('bhs,bhsv,bhsk->bhvk', ws, vj, kj)
        n_new = sc[..., None] * n + jnp.einsum('bhs,bhsk->bhk', ws, kj)
        return (C_new, n_new, m_new), h

    init = (jnp.zeros((B, H, d, d), f32), jnp.zeros((B, H, d), f32), jnp.zeros((B, H), f32))
    _, h = lax.scan(step, init, xs)
    return jnp.moveaxis(h, 0, 2).reshape(B, H, S, d).astype(dtype)


def hier_moe(x, w_grp, b_grp, w_erouter, b_erouter, w1, w3, w2):
    T, D = x.shape
    g_logits = (x @ w_grp).astype(jnp.float32) + b_grp
    g_prob = jax.nn.softmax(g_logits, axis=-1)
    g_sel = jnp.argmax(g_logits, axis=-1).astype(jnp.int32)
    g_w = jnp.take_along_axis(g_prob, g_sel[:, None], axis=-1)[:, 0]
    e_all = jnp.einsum('td,gde->tge', x, w_erouter).astype(jnp.float32) + b_erouter
    e_logits = jnp.take_along_axis(e_all, g_sel[:, None, None], axis=1)[:, 0]
    top_v, top_i = lax.top_k(e_logits, TOP_K)
    e_w = jax.nn.softmax(top_v, axis=-1) * g_w[:, None]
    e_id = g_sel[:, None] * EXPERTS_PER_GROUP + top_i.astype(jnp.int32)

    A = T * TOP_K
    flat_e = e_id.reshape(A)
    flat_tok = jnp.repeat(jnp.arange(T, dtype=jnp.int32), TOP_K)
    flat_w = e_w.reshape(A)
    order = jnp.argsort(flat_e)
    se, stok, sw = flat_e[order], flat_tok[order], flat_w[order]
    counts = jnp.bincount(flat_e, length=N_EXPERTS).astype(jnp.int32)
    starts = jnp.cumsum(counts) - counts
    pcounts = (counts + MOE_BLOCK - 1) // MOE_BLOCK * MOE_BLOCK
    pend = jnp.cumsum(pcounts)
    pstart = pend - pcounts
    dest = pstart[se] + (jnp.arange(A, dtype=jnp.int32) - starts[se])
    P = A + N_EXPERTS * MOE_BLOCK
    nb = P // MOE_BLOCK
    tok_buf = jnp.zeros((P,), jnp.int32).at[dest].set(stok)
    w_buf = jnp.zeros((P,), x.dtype).at[dest].set(sw.astype(x.dtype))
    blk_e = jnp.minimum(jnp.searchsorted(pend, jnp.arange(nb, dtype=jnp.int32) * MOE_BLOCK, side='right'),
                        N_EXPERTS - 1)
    xb = x[tok_buf].reshape(nb, MOE_BLOCK, D)

    def expert_block(args):
        xi, e = args
        hdn = jax.nn.silu(xi @ w1[e]) * (xi @ w3[e])
        return hdn @ w2[e]

    yb = lax.map(expert_block, (xb, blk_e)).reshape(P, D)
    return jnp.zeros_like(x).at[tok_buf].add(yb * w_buf[:, None])


def setup_inputs(seed: int = 0) -> dict:
    key = jax.random.key(seed)
    ks = jax.random.split(key, 24)
    f32 = jnp.float32

    def nrm(k, shape, s):
        return jax.random.normal(k, shape, f32) * s

    x = nrm(ks[0], (BATCH, SEQ, D_MODEL), 1.0)
    w_in = nrm(ks[1], (DEPTH, D_MODEL, D_IN), D_MODEL ** -0.5)
    conv_w = nrm(ks[2], (DEPTH, CONV_K, 2 * ML_WIDTH), CONV_K ** -0.5)
    conv_b = nrm(ks[3], (DEPTH, 2 * ML_WIDTH), 0.01)
    i_b = nrm(ks[4], (DEPTH, 1, ML_HEADS), 0.1)
    f_b = 3.0 + 3.0 * jax.random.uniform(ks[5], (DEPTH, 1, ML_HEADS), f32)
    gate_b = jnp.concatenate([i_b, f_b], axis=1)
    lam_qk = nrm(ks[6], (DEPTH, 4, DA_HEAD_DIM), 0.1)
    subln_g = 1.0 + nrm(ks[7], (DEPTH, DA_V_DIM), 0.02)
    mhnorm_g = 1.0 + nrm(ks[8], (DEPTH, ML_HEADS, ML_HEAD_DIM), 0.02)
    w_out = nrm(ks[9], (DEPTH, D_MIX, D_MODEL), D_MIX ** -0.5)
    g_mix = 1.0 + nrm(ks[10], (DEPTH, D_MODEL), 0.02)
    g_ffn = 1.0 + nrm(ks[11], (DEPTH, D_MODEL), 0.02)
    w_grp = nrm(ks[12], (DEPTH, D_MODEL, N_GROUPS), D_MODEL ** -0.5)
    b_grp = nrm(ks[13], (DEPTH, N_GROUPS), 0.01)
    w_erouter = nrm(ks[14], (DEPTH, N_GROUPS, D_MODEL, EXPERTS_PER_GROUP), D_MODEL ** -0.5)
    b_erouter = nrm(ks[15], (DEPTH, N_GROUPS, EXPERTS_PER_GROUP), 0.01)
    w1 = nrm(ks[16], (DEPTH, N_EXPERTS, D_MODEL, D_EXPERT), D_MODEL ** -0.5)
    w3 = nrm(ks[17], (DEPTH, N_EXPERTS, D_MODEL, D_EXPERT), D_MODEL ** -0.5)
    w2 = nrm(ks[18], (DEPTH, N_EXPERTS, D_EXPERT, D_MODEL), D_EXPERT ** -0.5)
    g_final = 1.0 + nrm(ks[19], (D_MODEL,), 0.02)
    return {'x': x, 'w_in': w_in, 'conv_w': conv_w, 'conv_b': conv_b, 'gate_b': gate_b,
            'lam_qk': lam_qk, 'subln_g': subln_g, 'mhnorm_g': mhnorm_g, 'w_out': w_out,
            'g_mix': g_mix, 'g_ffn': g_ffn, 'w_grp': w_grp, 'b_grp': b_grp,
            'w_erouter': w_erouter, 'b_erouter': b_erouter, 'w1': w1, 'w3': w3, 'w2': w2,
            'g_final': g_final}


def reference(x, w_in, conv_w, conv_b, gate_b, lam_qk, subln_g, mhnorm_g, w_out,
              g_mix, g_ffn, w_grp, b_grp, w_erouter, b_erouter, w1, w3, w2, g_final):
    B, S, D = x.shape
    cos, sin = rope_tables(S, DA_HEAD_DIM)
    for l in range(DEPTH):
        h = rmsnorm(x, g_mix[l])
        proj = h @ w_in[l]
        qa, ka, va, qm, km, vm, om, ig, fg = split_cols(proj)

        lambda_init = 0.8 - 0.6 * math.exp(-0.3 * l)
        lq = lam_qk[l].astype(jnp.float32)
        lam = jnp.exp(jnp.sum(lq[0] * lq[1])) - jnp.exp(jnp.sum(lq[2] * lq[3])) + lambda_init
        qa = apply_rope(qa.reshape(B, S, DA_HEADS, 2, DA_HEAD_DIM).transpose(0, 2, 3, 1, 4), cos, sin)
        ka = apply_rope(ka.reshape(B, S, DA_HEADS, 2, DA_HEAD_DIM).transpose(0, 2, 3, 1, 4), cos, sin)
        va = va.reshape(B, S, DA_HEADS, DA_V_DIM).transpose(0, 2, 1, 3)
        oa = diff_attention(qa, ka, va, lam)
        oa = rmsnorm(oa, subln_g[l]) * (1.0 - lambda_init)
        oa = oa.transpose(0, 2, 1, 3).reshape(B, S, DA_WIDTH)

        qk = jax.nn.silu(causal_conv(jnp.concatenate([qm, km], axis=-1), conv_w[l], conv_b[l]))
        qm, km = qk[..., :ML_WIDTH], qk[..., ML_WIDTH:]

        def heads(t):
            return t.reshape(B, S, ML_HEADS, ML_HEAD_DIM).transpose(0, 2, 1, 3)

        i_pre = (ig + gate_b[l, 0]).transpose(0, 2, 1)
        f_pre = (fg + gate_b[l, 1]).transpose(0, 2, 1)
        hm = mlstm_chunkwise(heads(qm), heads(km), heads(vm), i_pre, f_pre)
        hm = hm.transpose(0, 2, 1, 3) * jax.nn.sigmoid(om.reshape(B, S, ML_HEADS, ML_HEAD_DIM))
        hm = rmsnorm(hm, mhnorm_g[l]).reshape(B, S, ML_WIDTH)

        x = x + jnp.concatenate([oa, hm], axis=-1) @ w_out[l]

        hn = rmsnorm(x, g_ffn[l]).reshape(B * S, D)
        y = hier_moe(hn, w_grp[l], b_grp[l], w_erouter[l], b_erouter[l], w1[l], w3[l], w2[l])
        x = x + y.reshape(B, S, D)
    return rmsnorm(x, g_final)
```

```python
import math
from contextlib import ExitStack

import numpy as np
import concourse.bass as bass
import concourse.mybir as mybir
from concourse.bass_utils import run_bass_kernel_spmd

F32 = mybir.dt.float32
BF16 = mybir.dt.bfloat16
I32 = mybir.dt.int32
U32 = mybir.dt.uint32
AF = mybir.ActivationFunctionType
ALU = mybir.AluOpType
AX = mybir.AxisListType

NCORES = 8
D = 1024
S = 2048
NSEQ = 2
TOK = NSEQ * S
D_IN = 3592
EPS = 1e-6
LAMBDA_INIT = 0.2
NEXP = 32
DEXP = 512
CAP = 1024
GRP = 256

ENG_NAMES = ("pe", "act", "dve", "pool", "sp")


class _Op:
    __slots__ = ("eng", "fn", "deps", "is_dma", "dsem", "dval", "sig", "sigval", "idx", "waits", "cond")

    def __init__(self, eng, fn, is_dma):
        self.eng = eng
        self.fn = fn
        self.deps = set()
        self.is_dma = is_dma
        self.dsem = None
        self.dval = 0
        self.sig = False
        self.sigval = 0
        self.waits = None
        self.cond = None


_ESZ = {}


def _esize(dt):
    r = _ESZ.get(dt)
    if r is None:
        n = str(dt)
        r = 4 if "32" in n else (2 if "16" in n else (8 if "64" in n else 1))
        _ESZ[dt] = r
    return r


def _region(ap):
    r = _region0(ap)
    e = _esize(ap.dtype)
    lo, hi = r[3] * e, r[4] * e
    if str(ap.space) == "PSUM":
        return (r[0], 0, 128, (lo // 2048) * 2048, ((hi + 2047) // 2048) * 2048)
    return (r[0], r[1], r[2], lo, hi)


def _region0(ap):
    name = ap.tensor.name
    dims = ap.ap
    off = int(ap.offset)
    if str(ap.space) == "DRAM":
        lo = hi = off
        for st, cnt in dims:
            if cnt > 1:
                if st >= 0:
                    hi += st * (cnt - 1)
                else:
                    lo += st * (cnt - 1)
        return (name, 0, 1, lo, hi + 1)
    pst, pcnt = dims[0]
    assert pst > 0, "partition-broadcast SBUF APs not supported by tracker"
    p0 = off // pst
    lo = hi = off % pst
    for st, cnt in dims[1:]:
        if cnt > 1:
            if st >= 0:
                hi += st * (cnt - 1)
            else:
                lo += st * (cnt - 1)
    return (name, p0, p0 + pcnt, lo, hi + 1)


class Tracker:
    def __init__(self, nc, n_dma_sems=20):
        self.nc = nc
        self.ops = []
        self.recs = {}
        self.n_dma_sems = n_dma_sems
        self.dma_rr = {}
        self.dma_last = {}
        self.cur_cond = None
        self.ncond = 0

    def begin_cond(self, pred_ap):
        self.ncond += 1
        self.cur_cond = (self.ncond, pred_ap)

    def end_cond(self):
        self.cur_cond = None

    def _access(self, op, idx, ap, is_write, tag=None):
        name, p0, p1, f0, f1 = _region(ap)
        lst = self.recs.get(name, ())
        keep = []
        for r in lst:
            overlap = not (r[1] <= p0 or p1 <= r[0] or r[3] <= f0 or f1 <= r[2])
            if overlap and r[4] != idx and (r[5] or is_write) and not (tag is not None and r[7] == tag):
                same = (r[6] == op.eng and not op.is_dma and not self.ops[r[4]].is_dma)
                if same:
                    if op.eng != "pe":
                        op.deps.add(r[4])
                else:
                    op.deps.add(r[4])
            if is_write and r[0] >= p0 and r[1] <= p1 and r[2] >= f0 and r[3] <= f1 and r[4] != idx \
                    and not (tag is not None and r[7] == tag):
                continue
            if (not is_write) and (not r[5]) and r[6] == op.eng and not op.is_dma \
                    and r[0] == p0 and r[1] == p1 and r[2] == f0 and r[3] == f1:
                continue
            keep.append(r)
        keep.append([p0, p1, f0, f1, idx, is_write, op.eng, tag])
        self.recs[name] = keep

    def op(self, eng, fn, reads=(), writes=()):
        o = _Op(eng, fn, False)
        idx = len(self.ops)
        o.idx = idx
        o.cond = self.cur_cond
        self.ops.append(o)
        if o.cond is not None:
            self._access(o, idx, o.cond[1], False)
        for ap in writes:
            self._access(o, idx, ap, True)
        for ap in reads:
            self._access(o, idx, ap, False)
        return o

    def dma(self, queue, out, in_, fn=None, extra_reads=(), out_tag=None):
        if fn is None:
            fn = lambda e, out=out, in_=in_: e.dma_start(out=out, in_=in_)
        o = _Op(queue, fn, True)
        idx = len(self.ops)
        o.idx = idx
        o.cond = self.cur_cond
        self.ops.append(o)
        if o.cond is not None:
            self._access(o, idx, o.cond[1], False)
        self._access(o, idx, in_, False)
        for ap in extra_reads:
            self._access(o, idx, ap, False)
        self._access(o, idx, out, True, tag=out_tag)
        k = self.dma_rr.get(queue, 0)
        self.dma_rr[queue] = (k + 1) % self.n_dma_sems
        o.dsem = (queue, k)
        prev = self.dma_last.get((queue, k))
        if prev is not None:
            o.deps.add(prev)
            o.dval = self.ops[prev].dval + 16
        else:
            o.dval = 16
        self.dma_last[(queue, k)] = idx
        return o

    def finalize(self, es):
        nc = self.nc
        ops = self.ops
        for o in ops:
            for d in o.deps:
                if not ops[d].is_dma:
                    ops[d].sig = True
        cnt = {}
        for o in ops:
            if not o.is_dma and o.sig:
                cnt[o.eng] = cnt.get(o.eng, 0) + 1
                o.sigval = cnt[o.eng]
        esem = {e: es.enter_context(nc.semaphore("sem_" + e)) for e in ("pe", "act", "dve", "pool")}
        dsem = {}
        for q in self.dma_rr:
            for k in range(self.n_dma_sems):
                dsem[(q, k)] = es.enter_context(nc.semaphore("dsem_%s_%d" % (q, k)))
        streams = {e: [] for e in ENG_NAMES}
        water = {e: {} for e in ENG_NAMES}
        for o in ops:
            need = {}
            for d in o.deps:
                p = ops[d]
                if p.is_dma:
                    key = ("d",) + p.dsem
                    val = p.dval
                else:
                    key = ("e", p.eng)
                    val = p.sigval
                if val > need.get(key, 0):
                    need[key] = val
            w = []
            wm = water[o.eng]
            for key, val in need.items():
                if wm.get(key, 0) >= val:
                    continue
                wm[key] = val
                sem = esem[key[1]] if key[0] == "e" else dsem[(key[1], key[2])]
                w.append((sem, val))
            o.waits = w
            streams[o.eng].append(o)
        final_waits = [(q, dsem[(q, k)], ops[i].dval) for (q, k), i in self.dma_last.items()]
        self.stats = {e: len(streams[e]) for e in ENG_NAMES}
        block = es.enter_context(nc.Block())

        def emit_op(engine, o, with_waits=True):
            if with_waits:
                for sem, val in o.waits:
                    engine.wait_ge(sem, val)
            ins = o.fn(engine)
            if o.is_dma:
                ins.then_inc(dsem[o.dsem], 16)
            elif o.sig:
                ins.then_inc(esem[o.eng], 1)

        def emit(engine, lst, qname):
            i = 0
            reg = None
            while i < len(lst):
                o = lst[i]
                if o.cond is None:
                    emit_op(engine, o)
                    i += 1
                    continue
                j = i
                while j < len(lst) and lst[j].cond is not None and lst[j].cond[0] == o.cond[0]:
                    j += 1
                run = lst[i:j]
                for sem, val in o.waits:
                    engine.wait_ge(sem, val)
                if reg is None:
                    reg = engine.alloc_register("cnd_" + qname)
                engine.reg_load(reg, o.cond[1])
                with engine.If_ne(reg, 0):
                    for k, r in enumerate(run):
                        emit_op(engine, r, with_waits=(k > 0))
                with engine.Else():
                    nsig = 0
                    first_sig = None
                    for r in run:
                        if r.is_dma:
                            if r.dval > 16:
                                engine.wait_ge(dsem[r.dsem], r.dval - 16)
                            engine.sem_inc(dsem[r.dsem], 16)
                        elif r.sig:
                            nsig += 1
                            if first_sig is None:
                                first_sig = r.sigval
                    if nsig:
                        if first_sig > 1:
                            engine.wait_ge(esem[run[0].eng], first_sig - 1)
                        engine.sem_inc(esem[run[0].eng], nsig)
                i = j
            for q, sem, val in final_waits:
                if q == qname:
                    engine.wait_ge(sem, val)

        @block.tensor
        def _(e):
            emit(e, streams["pe"], "pe")

        @block.scalar
        def _(e):
            emit(e, streams["act"], "act")

        @block.vector
        def _(e):
            emit(e, streams["dve"], "dve")

        @block.gpsimd
        def _(e):
            emit(e, streams["pool"], "pool")

        @block.sync
        def _(e):
            emit(e, streams["sp"], "sp")


ARENA_BYTES = 122880
KSCALE_LN = math.log(128.0 ** -0.5)
ZR = NEXP * CAP


class Arena:
    def __init__(self, b, nbytes):
        self.t = b.sb("AR", [128, nbytes // 2], BF16)
        self.n = nbytes
        self.off = 0
        self.peak = 0

    def mark(self):
        return self.off

    def release(self, m):
        self.off = m

    def alloc(self, shape, dt=F32):
        esz = 2 if dt == BF16 else 4
        nfree = 1
        for d in shape[1:]:
            nfree *= d
        nb = nfree * esz
        start = self.off
        self.off += (nb + 63) // 64 * 64
        self.peak = max(self.peak, self.off)
        assert self.off <= self.n, ("arena overflow", self.off, self.n)
        v = self.t[0:shape[0], start // 2:(start + nb) // 2]
        if dt != BF16:
            v = v.bitcast(dt)
        if len(shape) > 2:
            names = ["a%d" % i for i in range(len(shape) - 1)]
            pat = "p (" + " ".join(names) + ") -> p " + " ".join(names)
            v = v.rearrange(pat, **{n: d for n, d in zip(names, shape[1:])})
        return v


def bcast_free(ap, n):
    return bass.AP(ap.tensor, ap.offset, [[ap.ap[0][0], ap.ap[0][1]], [0, n]])


class StopBuild(Exception):
    pass


class Builder:
    def __init__(self, dbg=None, stop_after=None):
        self.dbg = dbg or ()
        self.stop_after = stop_after
        self.nc = bass.Bass("TRN2", target_bir_lowering=False)
        self.T = Tracker(self.nc)
        self.es = ExitStack()

    def din(self, name, shape, dt=F32):
        return self.nc.dram_tensor(name, list(shape), dt, kind="ExternalInput").ap()

    def dout(self, name, shape, dt=F32):
        return self.nc.dram_tensor(name, list(shape), dt, kind="ExternalOutput").ap()

    def dscratch(self, name, shape, dt=F32):
        return self.nc.dram_tensor(name, list(shape), dt, kind="Internal").ap()

    def sb(self, name, shape, dt=F32):
        return self.es.enter_context(self.nc.sbuf_tensor(name, list(shape), dt))

    def ps(self, name, shape, dt=F32):
        return self.es.enter_context(self.nc.psum_tensor(name, list(shape), dt))

    def mm(self, out, lhsT, rhs, start=True, stop=True):
        self.T.op("pe", lambda e: e.matmul(out, lhsT, rhs, start=start, stop=stop),
                  reads=[lhsT, rhs], writes=[out])

    def tr(self, out, in_, ident):
        self.T.op("pe", lambda e: e.transpose(out, in_, ident), reads=[in_, ident], writes=[out])

    def act(self, out, in_, func, bias=None, scale=None, accum=None):
        reads = [in_]
        kw = {}
        if bias is not None:
            kw["bias"] = bias
            if not isinstance(bias, (int, float)):
                reads.append(bias)
        if scale is not None:
            kw["scale"] = scale
            if not isinstance(scale, (int, float)):
                reads.append(scale)
        writes = [out]
        if accum is not None:
            kw["accum_out"] = accum
            writes.append(accum)
        self.T.op("act", lambda e: e.activation(out, in_, func, **kw), reads=reads, writes=writes)

    def tt(self, out, a, b, op, eng="dve"):
        self.T.op(eng, lambda e: e.tensor_tensor(out, a, b, op), reads=[a, b], writes=[out])

    def ts(self, out, a, s1, s2=None, op0=ALU.mult, op1=None, eng="dve"):
        reads = [a]
        for s in (s1, s2):
            if s is not None and not isinstance(s, (int, float)):
                reads.append(s)
        kw = {}
        if op1 is not None:
            kw["op1"] = op1
        self.T.op(eng, lambda e: e.tensor_scalar(out, a, s1, s2, op0, **kw), reads=reads, writes=[out])

    def stt(self, out, a, scalar, b, op0, op1, eng="dve"):
        reads = [a, b]
        if not isinstance(scalar, (int, float)):
            reads.append(scalar)
        self.T.op(eng, lambda e: e.scalar_tensor_tensor(out, a, scalar, b, op0, op1),
                  reads=reads, writes=[out])

    def copy(self, out, in_, eng="dve"):
        if eng == "act":
            self.T.op("act", lambda e: e.copy(out, in_), reads=[in_], writes=[out])
        else:
            self.T.op(eng, lambda e: e.tensor_copy(out, in_), reads=[in_], writes=[out])

    def rsqrt(self, out, in_, c):
        self.act(out, in_, AF.Ln, bias=float(c))
        self.act(out, out, AF.Exp, scale=-0.5)

    def recip(self, out, in_):
        self.T.op("dve", lambda e: e.reciprocal(out, in_), reads=[in_], writes=[out])

    def reduce(self, out, in_, op, axis=AX.X):
        self.T.op("dve", lambda e: e.tensor_reduce(out, in_, axis, op), reads=[in_], writes=[out])

    def scan(self, out, d0, d1, init, op0, op1, d0_reads=()):
        self.T.op("dve", lambda e: e.tensor_tensor_scan(out, d0, d1, init, op0, op1),
                  reads=list(d0_reads) + [d1], writes=[out])

    def memset(self, ap, val, eng="dve"):
        self.T.op(eng, lambda e: e.memset(ap, val), reads=[], writes=[ap])

    def dma(self, out, in_, q="sp"):
        self.T.dma(q, out, in_)

    def scatter_rows(self, dram, idx_ap, src, bound):
        def fn(e):
            if getattr(self, "_bnd_reg", None) is None:
                self._bnd_reg = e.alloc_register("bnd")
                e.reg_mov(self._bnd_reg, bound)
            return e.indirect_dma_start(
                out=dram, out_offset=bass.IndirectOffsetOnAxis(ap=idx_ap, axis=0), in_=src, in_offset=None,
                bounds_check=self._bnd_reg, oob_is_err=False)
        self.T.dma("pool", dram, src, fn=fn, extra_reads=[idx_ap], out_tag="disjoint_rows")

    def gather_rows(self, dst, dram, idx_ap):
        self.T.dma("pool", dst, dram, fn=lambda e: e.indirect_dma_start(
            out=dst, out_offset=None, in_=dram, in_offset=bass.IndirectOffsetOnAxis(ap=idx_ap, axis=0)),
            extra_reads=[idx_ap])

    def chk(self, name):
        if self.stop_after == name:
            raise StopBuild()

    def PS(self, i):
        return self.PSB[i // 2][:, (i % 2) * 512:(i % 2 + 1) * 512]

    def build(self):
        nc, T = self.nc, self.T
        din, sb = self.din, self.sb
        self.x = din("x", [TOK, D])
        self.w_in = din("w_in", [D, D_IN])
        self.w_out = din("w_out", [D, D])
        self.w1 = din("w1", [NEXP, D, DEXP])
        self.w3 = din("w3", [NEXP, D, DEXP])
        self.w2 = din("w2", [NEXP, DEXP, D])
        c_rope = din("c_rope", [128, 2, S])
        self.c_rope = c_rope
        c_ident = din("c_ident", [128, 128])
        c_tri = din("c_tri", [128, 128])
        c_lstrict = din("c_lstrict", [128, 128])
        c_maskneg = din("c_maskneg", [128, 128])
        c_oh = din("c_oh", [128, 4, 128])
        c_sel = din("c_sel", [128, 64])
        c_iota = din("c_iota", [128, 32])
        lam_d = din("lam_qk", [4, 64])
        subln_d = din("subln_g", [128, 1])
        gmix_d = din("g_mix", [128, 8])
        convw_d = din("conv_w", [128, 8, 4])
        convb_d = din("conv_b", [128, 8])
        gateb_d = din("gate_b", [4, 2])
        mhg_d = din("mhg_bc", [128, 512])
        gffn_d = din("gffn_bc", [128, D])
        gfin_d = din("gfin_bc", [128, D])
        gffnc_d = din("gffn_col", [128, 8])
        wr_d = din("wr", [128, 8, 36])
        rbias_d = din("rbias", [128, 36])
        self.out = self.dout("out", [TOK, D])
        self.x1_d = self.dscratch("x1_d", [TOK, D])
        self.xg_d = self.dscratch("xg_d", [NEXP * CAP, D], BF16)
        self.y_d = self.dscratch("y_d", [NEXP * CAP + 1, D], BF16)

        self.ident_f = sb("ident_f", [128, 128])
        self.ident_b = sb("ident_b", [128, 128], BF16)
        self.tri_b = sb("tri_b", [128, 128], BF16)
        self.lstrict_b = sb("lstrict_b", [128, 128], BF16)
        self.ones_b = sb("ones_b", [128, 128], BF16)
        self.maskneg = sb("maskneg", [128, 128])
        self.OH = sb("OH", [128, 4, 128])
        self.sel = sb("sel", [128, 64])
        self.iota_e = sb("iota_e", [128, 32])
        self.gmix = sb("gmix", [128, 8])
        self.neglam = sb("neglam", [128, 1])
        self.subg = sb("subg", [128, 1])
        self.convw = sb("convw", [128, 8, 4])
        self.convb = sb("convb", [128, 8])
        self.gateb = sb("gateb", [4, 2])
        self.negfb = sb("negfb", [4, 1])
        self.ones4 = sb("ones4", [4, 1])
        self.mhg = sb("mhg", [128, 512])
        self.gffn = sb("gffn", [128, D])
        self.gfin = sb("gfin", [128, D])
        self.gffnc = sb("gffnc", [128, 8])
        self.wr = sb("wr_t", [128, 8, 36])
        self.rbias = sb("rbias_t", [128, 36])
        self.carry = sb("carry", [128, 32])
        self.posg = sb("posg", [128, 32, 2], I32)
        self.wts = sb("wts", [128, 32, 2])
        self.hT = sb("hT", [128, 8, S], BF16)
        self.mixT = sb("mixT", [128, 8, S], BF16)
        self.arena = Arena(self, ARENA_BYTES)
        A = self.arena
        self.PSB = [self.ps("psb%d" % i, [128, 1024]) for i in range(4)]

        m = A.mark()
        tf = A.alloc([128, 128])
        for dst, src in ((self.tri_b, c_tri), (self.lstrict_b, c_lstrict), (self.ident_b, c_ident)):
            self.dma(tf, src)
            self.copy(dst[:], tf)
        self.dma(self.ident_f[:], c_ident)
        self.dma(self.maskneg[:], c_maskneg)
        self.dma(self.OH[:], c_oh)
        self.dma(self.sel[:], c_sel)
        self.dma(self.iota_e[:], c_iota)
        self.dma(self.gmix[:], gmix_d)
        self.dma(self.subg[:], subln_d)
        self.dma(self.convw[:], convw_d)
        self.dma(self.convb[:], convb_d)
        self.dma(self.gateb[:], gateb_d)
        self.dma(self.mhg[:], mhg_d)
        self.dma(self.gffn[:], gffn_d)
        self.dma(self.gfin[:], gfin_d)
        self.dma(self.gffnc[:], gffnc_d)
        self.dma(self.wr[:], wr_d)
        self.dma(self.rbias[:], rbias_d)
        self.memset(self.ones_b[:], 1.0)
        self.memset(self.ones4[:], 1.0)
        self.memset(self.carry[:], 0.0)
        sqD = float(math.sqrt(D))
        self.ts(self.gmix[:], self.gmix[:], sqD, None, op0=ALU.mult)
        self.ts(self.gffn[:], self.gffn[:], sqD, None, op0=ALU.mult)
        self.ts(self.gfin[:], self.gfin[:], sqD, None, op0=ALU.mult)
        self.ts(self.mhg[:], self.mhg[:], float(math.sqrt(128.0)), None, op0=ALU.mult)
        self.ts(self.subg[:], self.subg[:], float((1.0 - LAMBDA_INIT) * math.sqrt(128.0)), None, op0=ALU.mult)
        self.ts(self.negfb[:], self.gateb[:, 1:2], -1.0, None, op0=ALU.mult)
        self.ts(self.gffnc[:], self.gffnc[:], sqD, None, op0=ALU.mult)
        for c in range(8):
            self.ts(self.wr[:, c, :], self.wr[:, c, :], self.gffnc[:, c:c + 1], None, op0=ALU.mult)
        lam_t = A.alloc([128, 256])
        lam_p = A.alloc([128, 2, 64])
        lam_s = A.alloc([128, 2])
        self.dma(lam_t, lam_d.rearrange("a b -> (a b)").partition_broadcast(128))
        lv = lam_t.rearrange("p (a b c) -> p a b c", a=2, b=2)
        self.tt(lam_p, lv[:, :, 0, :], lv[:, :, 1, :], ALU.mult)
        self.reduce(lam_s, lam_p, ALU.add)
        self.act(lam_s, lam_s, AF.Exp)
        self.tt(self.neglam[:], lam_s[:, 1:2], lam_s[:, 0:1], ALU.subtract)
        self.ts(self.neglam[:], self.neglam[:], -LAMBDA_INIT, None, op0=ALU.add)
        zrow = A.alloc([1, D], BF16)
        self.memset(zrow, 0.0)
        self.dma(self.y_d[ZR:ZR + 1, :], zrow)
        A.release(m)

        self.w_in_v = self.w_in.rearrange("(c p) n -> p c n", p=128)
        done = False
        try:
            for s in range(NSEQ):
                m = A.mark()
                self.phase_norm(s)
                self.phase_attn(s)
                A.release(m)
                self.chk("ATT")
                self.phase_mlstm(s)
                A.release(m)
                self.chk("ML")
                self.phase_outproj(s)
                A.release(m)
                self.chk("C")
        except StopBuild:
            done = True
        if not done:
            self.phase_moe()
            A.release(0)
            self.phase_combine()

        dbg = self.dbg
        if "mixT" in dbg:
            d = self.dout("dbg_mixT", [128, 8, S], BF16)
            self.dma(d, self.mixT[:])
        if "route" in dbg:
            d = self.dout("dbg_posg", [128, 32, 2], I32)
            self.dma(d, self.posg[:])
            d = self.dout("dbg_wts", [128, 32, 2])
            self.dma(d, self.wts[:])
        if "x1" in dbg:
            d = self.dout("dbg_x1", [S, D])
            self.dma(d, self.x1_d[0:S, :])
        if done:
            zt = self.sb("zt_dummy", [128, D])
            self.memset(zt[:], 0.0)
            self.dma(self.out[0:128, :], zt[:])
        T.finalize(self.es)
        return nc

    def phase_norm(self, s):
        A = self.arena
        xt = A.alloc([128, 2, D])
        xsq = A.alloc([128, D], BF16)
        xs = A.alloc([128, 2, D], BF16)
        ssq = A.alloc([128, 2])
        rstd = A.alloc([128, 2])
        hT = self.hT
        for t in range(16):
            b = t % 2
            r0 = s * S + t * 128
            self.dma(xt[:, b, :], self.x[r0:r0 + 128, :])
            self.act(xsq, xt[:, b, :], AF.Square, accum=ssq[:, b:b + 1])
            self.rsqrt(rstd[:, b:b + 1], ssq[:, b:b + 1], D * EPS)
            self.ts(xs[:, b, :], xt[:, b, :], rstd[:, b:b + 1], None, op0=ALU.mult)
            pst = self.PS(b).bitcast(BF16)
            for c in range(8):
                self.tr(pst[:, c * 128:(c + 1) * 128], xs[:, b, c * 128:(c + 1) * 128], self.ident_b[:])
            self.tt(hT[:, :, t * 128:(t + 1) * 128], pst.rearrange("p (c t) -> p c t", c=8),
                    self.gmix[:, :].unsqueeze(2).to_broadcast([128, 8, 128]), ALU.mult)

    def phase_attn(self, s):
        A = self.arena
        hT = self.hT
        rope_t = A.alloc([128, 2, S])
        wv = A.alloc([128, 8, 512], BF16)
        wqk = A.alloc([128, 2, 2, 8, 128], BF16)
        wsw = A.alloc([128, 2, 2, 8, 128], BF16)
        QT2 = A.alloc([128, 2, S], BF16)
        KT2 = A.alloc([128, 2, S], BF16)
        Vt = A.alloc([128, 16, 512], BF16)
        PT = A.alloc([128, 4, 512], BF16)
        rp_a = A.alloc([128, 512])
        rp_b = A.alloc([128, 512])
        tmp = (A.alloc([128, 512]), A.alloc([128, 512]), A.alloc([128, 512]), A.alloc([128, 512], BF16),
               A.alloc([128, 512]))
        sqt = A.alloc([128, 2, 512], BF16)
        bdiag = A.alloc([128, 128], BF16)
        selcf = A.alloc([128, 2, 128])
        nmax = A.alloc([128, 2, 2, 4])
        nm2 = A.alloc([128, 2])
        prod32 = A.alloc([128, 32])
        negM2 = A.alloc([128, 2, 2])
        self.memset(bdiag, 0.0)
        self.memset(bdiag[0:64, 0:64], 1.0)
        self.memset(bdiag[64:128, 64:128], 1.0)
        self.memset(selcf, 0.0)
        self.memset(selcf[0:64, 0, :], 1.0 / 64.0)
        self.memset(selcf[64:128, 1, :], 1.0 / 64.0)
        self.dma(rope_t, self.c_rope)
        self.dma(wv, self.w_in_v[:, :, 1024:1536], q="pool")
        for t in range(16):
            pb = self.PS(2 + t % 2)
            for c in range(8):
                self.mm(pb, hT[:, c, t * 128:(t + 1) * 128], wv[:, c, :], start=(c == 0), stop=(c == 7))
            self.copy(Vt[:, t, :], pb, eng=("act" if t % 2 == 0 else "dve"))

        def prep(h):
            bf = h % 2
            self.dma(wqk[:, bf, 0, :, :], self.w_in_v[:, :, h * 128:(h + 1) * 128], q="pool")
            self.dma(wqk[:, bf, 1, :, :], self.w_in_v[:, :, 512 + h * 128:512 + (h + 1) * 128], q="pool")
            for j in range(2):
                src = wqk[:, bf, j, :, :].rearrange("p k (c f i) -> p k c f i", c=2, f=2)
                dst = wsw[:, bf, j, :, :].rearrange("p k (c f i) -> p k c f i", c=2, f=2)
                for f in range(2):
                    self.copy(dst[:, :, :, f, :], src[:, :, :, 1 - f, :], eng="pool")
            pend = None

            def norms(jn):
                j, n = jn
                pn = self.PS(2 + n % 2)
                self.mm(pn, bdiag, sqt[:, n % 2, :])
                self.reduce(nmax[:, bf, j, n:n + 1], pn, ALU.max)

            for j, dstT in ((0, QT2), (1, KT2)):
                for n in range(4):
                    p1 = self.PS(4 + (n % 2) * 2)
                    p2 = self.PS(5 + (n % 2) * 2)
                    for c in range(8):
                        self.mm(p1, wqk[:, bf, j, c, :], hT[:, c, n * 512:(n + 1) * 512], start=(c == 0), stop=(c == 7))
                    for c in range(8):
                        self.mm(p2, wsw[:, bf, j, c, :], hT[:, c, n * 512:(n + 1) * 512], start=(c == 0), stop=(c == 7))
                    if pend is not None:
                        norms(pend)
                    self.tt(rp_a, p1, rope_t[:, 0, n * 512:(n + 1) * 512], ALU.mult)
                    self.tt(rp_b, p2, rope_t[:, 1, n * 512:(n + 1) * 512], ALU.mult)
                    self.tt(dstT[:, bf, n * 512:(n + 1) * 512], rp_a, rp_b, ALU.add)
                    self.tt(sqt[:, n % 2, :], dstT[:, bf, n * 512:(n + 1) * 512], dstT[:, bf, n * 512:(n + 1) * 512],
                            ALU.mult)
                    pend = (j, n)
            norms(pend)
            self.reduce(nm2, nmax[:, bf, :, :], ALU.max)
            self.tt(prod32[:, 0:1], nm2[:, 0:1], nm2[:, 1:2], ALU.mult)
            self.copy(prod32[:, 1:32], prod32[:, 0:1].to_broadcast([128, 31]))
            pm = self.PS(2)
            for c in range(2):
                self.mm(pm[:, c * 32:(c + 1) * 32], selcf[:, c, :], prod32)
            negM = negM2[:, bf, :]
            self.act(negM, pm[:, 0:64].rearrange("p (c n) -> p c n", c=2)[:, :, 0], AF.Ln)
            self.act(negM, negM, AF.Exp, scale=0.5)
            self.ts(negM, negM, -1.0 / 8.0, None, op0=ALU.mult)

        prep(0)
        for h in range(4):
            if h + 1 < 4:
                prep(h + 1)
            bf = h % 2
            self.attention(h, QT2[:, bf, :], KT2[:, bf, :], Vt, PT, tmp, negM2[:, bf, :])

    def attention(self, h, QT, KT, Vt, PT, tmp, negM):
        e_r, e_a, e_b, e_sq, e_rs = tmp
        PS = self.PS
        SB = [PS(0), PS(1), PS(2)]
        OT = [PS(3), PS(4)]
        SS = [PS(5), PS(6)]
        SQ = PS(7)
        mixT = self.mixT
        ones_b, tri_b, neglam, subg = self.ones_b, self.tri_b, self.neglam, self.subg
        scale = 1.0 / 8.0
        steps = []
        for qc in range(4):
            last = 4 * qc + 3
            for kt in range(last + 1):
                if kt < 4 * qc:
                    q0, n = qc * 512, 512
                else:
                    j = kt - 4 * qc
                    q0, n = qc * 512 + j * 128, 512 - j * 128
                steps.append((qc, kt, q0, n, kt == 0, kt == last))

        def qk_exp(i):
            qc, kt, q0, n, first, lastf = steps[i]
            for c in range(2):
                sbk = SB[(2 * i + c) % 3]
                pslot = (2 * i + c) % 4
                self.mm(sbk[:, 0:n], KT[c * 64:(c + 1) * 64, kt * 128:(kt + 1) * 128],
                        QT[c * 64:(c + 1) * 64, q0:q0 + n])
                self.act(PT[:, pslot, 0:n], sbk[:, 0:n], AF.Exp, scale=scale, bias=negM[:, c:c + 1])
                if kt >= 4 * qc:
                    self.tt(PT[:, pslot, 0:128], PT[:, pslot, 0:128], tri_b[:], ALU.mult)

        def pv(i):
            qc, kt, q0, n, first, lastf = steps[i]
            off = q0 - qc * 512
            for c in range(2):
                pslot = (2 * i + c) % 4
                self.mm(OT[c][:, off:off + n], Vt[:, kt, h * 128:(h + 1) * 128], PT[:, pslot, 0:n],
                        start=first, stop=lastf)
                self.mm(SS[c][:, off:off + n], ones_b[:], PT[:, pslot, 0:n], start=first, stop=lastf)

        def epi1(qc):
            self.act(e_r, SS[0], AF.Ln)
            self.act(e_r, e_r, AF.Exp, scale=-1.0)
            self.act(e_rs, SS[1], AF.Ln)
            self.act(e_rs, e_rs, AF.Exp, scale=-1.0)
            self.tt(e_a, OT[0], e_r, ALU.mult)
            self.tt(e_b, OT[1], e_rs, ALU.mult)
            self.stt(e_a, e_b, neglam[:, 0:1], e_a, ALU.mult, ALU.add)
            self.tt(e_sq, e_a, e_a, ALU.mult)

        def epi2(qc):
            self.mm(SQ, ones_b[:], e_sq)
            self.rsqrt(e_rs, SQ, 128 * EPS)
            self.stt(mixT[:, h, qc * 512:(qc + 1) * 512], e_a, subg[:, 0:1], e_rs, ALU.mult, ALU.mult)

        qk_exp(0)
        pending = None
        for i in range(len(steps)):
            if i + 1 < len(steps):
                qk_exp(i + 1)
            pv(i)
            if pending is not None:
                epi2(pending)
                pending = None
            if steps[i][5]:
                epi1(steps[i][0])
                pending = steps[i][0]
        if pending is not None:
            epi2(pending)

    def phase_mlstm(self, s):
        A = self.arena
        PS = self.PS
        hT, mixT = self.hT, self.mixT
        w_in_v = self.w_in_v
        Vm = A.alloc([128, 16, 4, 129], BF16)
        sgo = A.alloc([128, 16, 512], BF16)
        qmT = A.alloc([128, 4, S], BF16)
        kmT = A.alloc([128, 4, S], BF16)
        RQa = A.alloc([128, S])
        cols = A.alloc([128, 16, 16])
        screp = A.alloc([128, 4, 16])
        mask4 = A.alloc([128, 4, 128])
        wqm = A.alloc([128, 2, 8, 128], BF16)
        wg = A.alloc([128, 8, 8], BF16)
        Mj = A.alloc([4, 17])
        sc = A.alloc([4, 64])
        m1 = A.mark()
        wvo = A.alloc([128, 8, 1024], BF16)
        self.dma(wvo, w_in_v[:, :, 2560:3584], q="pool")
        self.memset(Vm[:, :, :, 128:129], 1.0)
        for h in range(4):
            self.ts(mask4[:, h, :], self.tri_b[:], 1.0, None, op0=ALU.mult)
        for t in range(16):
            pa = PS(0 + 2 * (t % 2))
            pb = PS(1 + 2 * (t % 2))
            for c in range(8):
                self.mm(pa, hT[:, c, t * 128:(t + 1) * 128], wvo[:, c, 0:512], start=(c == 0), stop=(c == 7))
            for c in range(8):
                self.mm(pb, hT[:, c, t * 128:(t + 1) * 128], wvo[:, c, 512:1024], start=(c == 0), stop=(c == 7))
            self.copy(Vm[:, t, :, 0:128], pa.rearrange("p (h d) -> p h d", h=4), eng="dve")
            self.act(sgo[:, t, :], pb, AF.Sigmoid)
        A.release(m1)
        self.chk("ML_a")
        T0 = A.alloc([4, S])
        T1 = A.alloc([4, S])
        T2 = A.alloc([4, S])
        T3 = A.alloc([4, S])
        E = T1
        ones_row = bcast_free(self.ones4[:, 0:1], S)
        self.dma(wg, w_in_v[:, :, 3584:3592], q="pool")
        self.memset(RQa, 0.0)
        self.memset(sc, 0.0)
        for n in range(4):
            pi = PS(4 + 2 * (n % 2))
            pf = PS(5 + 2 * (n % 2))
            ns = slice(n * 512, (n + 1) * 512)
            for c in range(8):
                self.mm(pi[0:4, :], wg[:, c, 0:4], hT[:, c, ns], start=(c == 0), stop=(c == 7))
            for c in range(8):
                self.mm(pf[0:4, :], wg[:, c, 4:8], hT[:, c, ns], start=(c == 0), stop=(c == 7))
            self.ts(T0[:, ns], pi[0:4, :], self.gateb[:, 0:1], None, op0=ALU.add)
            self.act(E[:, ns], pf[0:4, :], AF.Exp, bias=self.negfb[:, 0:1], scale=-1.0)
        self.act(E, E, AF.Ln, bias=1.0)
        self.scan(T2, ones_row, E, 0.0, ALU.mult, ALU.add, d0_reads=[self.ones4[:]])
        self.tt(T0, T0, T2, ALU.add)
        self.scan(T3, ones_row, T0, 0.0, ALU.mult, ALU.max, d0_reads=[self.ones4[:]])
        self.tt(T1, T2, T3, ALU.subtract)
        self.act(T1, T1, AF.Exp)
        self.copy(RQa[96:100, :], T1)
        self.memset(Mj[:, 0:1], 0.0)
        T3v = T3.rearrange("p (j l) -> p j l", l=128)
        T0v = T0.rearrange("p (j l) -> p j l", l=128)
        T1v = T1.rearrange("p (j l) -> p j l", l=128)
        self.copy(Mj[:, 1:17], T3v[:, :, 127])
        self.tt(T1v, T3v, Mj[:, 0:16].unsqueeze(2).to_broadcast([4, 16, 128]), ALU.subtract)
        self.act(T1, T1, AF.Exp, scale=-1.0)
        self.copy(RQa[64:68, :], T1)
        self.tt(T1v, T3v, Mj[:, 1:17].unsqueeze(2).to_broadcast([4, 16, 128]), ALU.subtract)
        self.act(T1, T1, AF.Exp, scale=-1.0)
        self.copy(RQa[0:4, :], T1)
        self.tt(T1v, T0v, Mj[:, 1:17].unsqueeze(2).to_broadcast([4, 16, 128]), ALU.subtract)
        self.act(T1, T1, AF.Exp, bias=float(KSCALE_LN))
        self.copy(RQa[32:36, :], T1)
        self.tt(sc[:, 0:16], Mj[:, 0:16], Mj[:, 1:17], ALU.subtract)
        self.act(sc[:, 0:16], sc[:, 0:16], AF.Exp)
        self.chk("ML_b1")
        pc = self.PSB[0][:, :]
        for j in range(16):
            self.mm(pc[:, j * 64:(j + 1) * 64], RQa[:, j * 128:(j + 1) * 128], self.sel[:, :])
        self.copy(cols, pc.rearrange("p (j n) -> p j n", n=64)[:, :, 0:16])
        pr = PS(2)
        for h in range(4):
            self.mm(pr[:, h * 64:(h + 1) * 64], self.OH[0:4, h, :], sc[:, :])
        self.copy(screp, pr[:, 0:256].rearrange("p (h n) -> p h n", n=64)[:, :, 0:16])
        A.release(m1)
        self.chk("ML_b")
        xc = A.alloc([128, 2, S + 4])
        yb = A.alloc([128, 2, S])
        self.memset(xc[:, :, 0:3], 0.0)
        for h in range(4):
            self.dma(wqm[:, 0, :, :], w_in_v[:, :, 1536 + h * 128:1536 + (h + 1) * 128], q="pool")
            self.dma(wqm[:, 1, :, :], w_in_v[:, :, 2048 + h * 128:2048 + (h + 1) * 128], q="pool")
            for j in range(2):
                for n in range(4):
                    p = PS(4 + (2 * j + n) % 4)
                    for c in range(8):
                        self.mm(p, wqm[:, j, c, :], hT[:, c, n * 512:(n + 1) * 512], start=(c == 0), stop=(c == 7))
                    self.copy(xc[:, j, 3 + n * 512:3 + (n + 1) * 512], p, eng="act")
                ct = h + 4 * j
                self.ts(yb[:, j, :], xc[:, j, 3:3 + S], self.convw[:, ct, 3:4], self.convb[:, ct:ct + 1],
                        op0=ALU.mult, op1=ALU.add)
                for tap in (2, 1, 0):
                    self.stt(yb[:, j, :], xc[:, j, tap:tap + S], self.convw[:, ct, tap:tap + 1], yb[:, j, :],
                             ALU.mult, ALU.add)
                self.act((qmT if j == 0 else kmT)[:, h, :], yb[:, j, :], AF.Silu)
        A.release(m1)
        self.chk("ML_c")
        SmT = A.alloc([128, 512], BF16)
        num = A.alloc([128, 4, 129])
        t2 = A.alloc([128, 4, 129])
        hg = A.alloc([128, 4, 128])
        sq = A.alloc([128, 4, 128])
        ss = A.alloc([128, 4])
        rs = A.alloc([128, 4])
        den = A.alloc([128, 4])
        omix = A.alloc([128, 4, 128], BF16)
        wsv = A.alloc([128, 2, 4, 129], BF16)
        CT = A.alloc([128, 4, 129])
        CTb = A.alloc([128, 4, 129], BF16)
        ktok = A.alloc([128, 2, 4, 128], BF16)
        ST, TR = PS(0), PS(1)
        U, N1, N2 = self.PSB[1][:, :], self.PSB[2][:, :], self.PSB[3][:, :]
        trp = TR.bitcast(BF16)

        def v4(ps2):
            return ps2.rearrange("p (b r) -> p b r", b=2)[:, :, 0:258].rearrange("p b (g d) -> p b g d", g=2)

        def hv(ps2, h):
            o = (h // 2) * 512 + (h % 2) * 129
            return ps2[:, o:o + 129]

        N14, N24, U4 = v4(N1), v4(N2), v4(U)
        CT4 = CT.rearrange("p (b g) d -> p b g d", b=2)
        num4 = num.rearrange("p (b g) d -> p b g d", b=2)
        t24 = t2.rearrange("p (b g) d -> p b g d", b=2)
        ident_b = self.ident_b

        def col4(j, q):
            return cols[:, j, 4 * q:4 * q + 4].rearrange("p (b g) -> p b g", b=2).unsqueeze(3).to_broadcast(
                [128, 2, 2, 129])

        for j in range(16):
            js = slice(j * 128, (j + 1) * 128)
            kb = j % 2
            for h in range(4):
                self.tr(trp[:, h * 128:(h + 1) * 128], kmT[:, h, js], ident_b[:])
            self.copy(ktok[:, kb, :, :], trp[:, 0:512].rearrange("p (h t) -> p h t", h=4), eng="act")
            for h in range(4):
                self.mm(ST[:, h * 128:(h + 1) * 128], kmT[:, h, js], qmT[:, h, js])
            self.tt(wsv[:, kb, :, :], Vm[:, j, :, :], cols[:, j, 4:8].unsqueeze(2).to_broadcast([128, 4, 129]),
                    ALU.mult, eng="pool")
            self.tt(SmT, ST, mask4.rearrange("p h l -> p (h l)"), ALU.mult)
            for h in range(4):
                self.mm(hv(N1, h), SmT[:, h * 128:(h + 1) * 128], wsv[:, kb, h, :])
            if j > 0:
                for h in range(4):
                    self.mm(hv(N2, h), qmT[:, h, js], CTb[:, h, :])
            self.tt(num4, N14, col4(j, 0), ALU.mult)
            if j > 0:
                for h in range(4):
                    self.act(t2[:, h, :], hv(N2, h), AF.Copy, scale=cols[:, j, 8 + h:9 + h])
                self.tt(num, num, t2, ALU.add)
            self.stt(den, num[:, :, 128], -1.0, num[:, :, 128], ALU.mult, ALU.max)
            self.tt(den, den, cols[:, j, 12:16], ALU.max)
            self.recip(den, den)
            self.tt(hg, num[:, :, 0:128], den.unsqueeze(2).to_broadcast([128, 4, 128]), ALU.mult)
            self.tt(hg, hg, sgo[:, j, :].rearrange("p (h d) -> p h d", h=4), ALU.mult)
            for h in range(4):
                self.act(sq[:, h, :], hg[:, h, :], AF.Square, accum=ss[:, h:h + 1])
            self.rsqrt(rs, ss, 128 * EPS)
            self.tt(hg, hg, rs.unsqueeze(2).to_broadcast([128, 4, 128]), ALU.mult)
            self.tt(omix, hg, self.mhg[:, :].rearrange("p (h d) -> p h d", h=4), ALU.mult)
            for h in range(4):
                self.tr(trp[:, 512 + h * 128:512 + (h + 1) * 128], omix[:, h, :], ident_b[:])
            self.copy(mixT[:, 4:8, js], trp[:, 512:1024].rearrange("p (h t) -> p h t", h=4), eng="act")
            if j < 15:
                for h in range(4):
                    self.mm(hv(U, h), ktok[:, kb, h, :], wsv[:, kb, h, :])
                if j == 0:
                    self.copy(CT4, U4)
                else:
                    self.tt(CT, CT, screp[:, :, j:j + 1].to_broadcast([128, 4, 129]), ALU.mult)
                    self.tt(CT4, CT4, U4, ALU.add)
                self.copy(CTb, CT, eng="act")

    def phase_outproj(self, s):
        A = self.arena
        PS = self.PS
        mixT = self.mixT
        NT = 16
        wout = A.alloc([128, 8, D], BF16)
        xt = A.alloc([128, 2, D])
        x1 = A.alloc([128, 2, D])
        zall = A.alloc([128, NT, D], BF16)
        junk = A.alloc([128, D], BF16)
        x1T = A.alloc([128, 8, 128])
        ssq = A.alloc([128, 2])
        rstd = A.alloc([128, 2])
        Lall = A.alloc([128, NT, 36])
        self.dma(wout, self.w_out.rearrange("(c p) n -> p c n", p=128), q="pool")
        def outproj_tile(t):
            b = t % 2
            r0 = s * S + t * 128
            self.dma(xt[:, b, :], self.x[r0:r0 + 128, :])
            for half in range(2):
                p = PS(half + 2 * (t % 2))
                hs = slice(half * 512, (half + 1) * 512)
                for c in range(8):
                    self.mm(p, mixT[:, c, t * 128:(t + 1) * 128], wout[:, c, hs], start=(c == 0), stop=(c == 7))
                self.tt(x1[:, b, hs], p, xt[:, b, hs], ALU.add)
            self.dma(self.x1_d[r0:r0 + 128, :], x1[:, b, :])
            self.act(junk, x1[:, b, :], AF.Square, accum=ssq[:, b:b + 1])
            self.rsqrt(rstd[:, b:b + 1], ssq[:, b:b + 1], D * EPS)
            self.stt(zall[:, t, :], x1[:, b, :], rstd[:, b:b + 1], self.gffn[:], ALU.mult, ALU.mult)

        def router_tile(t):
            b = t % 2
            pT = self.PSB[2][:, :]
            for c in range(8):
                self.tr(pT[:, c * 128:(c + 1) * 128], x1[:, b, c * 128:(c + 1) * 128], self.ident_f[:])
            x1Tf = x1T.rearrange("p c t -> p (c t)")
            self.copy(x1Tf[:, 0:512], pT[:, 0:512], eng="act")
            self.copy(x1Tf[:, 512:1024], pT[:, 512:1024], eng="dve")
            pl = PS(6)
            for c in range(8):
                self.mm(pl[:, 0:36], x1T[:, c, :], self.wr[:, c, :], start=(c == 0), stop=(c == 7))
            self.stt(Lall[:, t, :], pl[:, 0:36], rstd[:, b:b + 1], self.rbias[:], ALU.mult, ALU.add)

        outproj_tile(0)
        for t in range(NT):
            if t + 1 < NT:
                outproj_tile(t + 1)
            router_tile(t)
        def col():
            return A.alloc([128, NT])
        gmax, gsum, gw, m1_, m2_, dd, e2, rr, wA, wB = [col() for _ in range(10)]
        ohg = A.alloc([128, NT, 4])
        gexp = A.alloc([128, NT, 4])
        t48 = A.alloc([128, NT, 4, 8])
        esel = A.alloc([128, NT, 8])
        esel2 = A.alloc([128, NT, 8])
        oh1 = A.alloc([128, NT, 8])
        oh2 = A.alloc([128, NT, 8])
        oe = [A.alloc([128, NT, 4, 8]), A.alloc([128, NT, 4, 8])]
        oeb = A.alloc([128, NT, 32], BF16)
        slot = A.alloc([128, NT, 32])
        t32 = A.alloc([128, NT, 32])
        sl, ei, ge, inv, pos = [col() for _ in range(5)]
        posi = A.alloc([128, NT, 2], I32)
        Lg4 = Lall[:, :, 0:4]

        def bc(c_, n):
            return c_.unsqueeze(2).to_broadcast([128, NT, n])

        self.reduce(gmax, Lg4, ALU.max)
        self.tt(ohg, Lg4, bc(gmax, 4), ALU.is_equal)
        self.tt(gexp, Lg4, bc(gmax, 4), ALU.subtract)
        self.act(gexp, gexp, AF.Exp)
        self.reduce(gsum, gexp, ALU.add)
        self.recip(gw, gsum)
        self.tt(t48, Lall[:, :, 4:36].rearrange("p t (g e) -> p t g e", g=4),
                ohg.unsqueeze(3).to_broadcast([128, NT, 4, 8]), ALU.mult)
        self.reduce(esel, t48.rearrange("p t g e -> p t e g"), ALU.add)
        self.reduce(m1_, esel, ALU.max)
        self.tt(oh1, esel, bc(m1_, 8), ALU.is_equal)
        self.stt(esel2, oh1, -1e30, esel, ALU.mult, ALU.add)
        self.reduce(m2_, esel2, ALU.max)
        self.tt(oh2, esel2, bc(m2_, 8), ALU.is_equal)
        self.tt(dd, m2_, m1_, ALU.subtract)
        self.act(e2, dd, AF.Exp)
        self.ts(rr, e2, 1.0, None, op0=ALU.add)
        self.recip(rr, rr)
        self.tt(wA, rr, gw, ALU.mult)
        self.tt(wB, e2, wA, ALU.mult)
        for k, ohk in ((0, oh1), (1, oh2)):
            self.copy(oe[k], ohk.unsqueeze(2).to_broadcast([128, NT, 4, 8]))
            self.tt(oe[k], oe[k], ohg.unsqueeze(3).to_broadcast([128, NT, 4, 8]), ALU.mult)
        oef = [o_.rearrange("p t g e -> p t (g e)") for o_ in oe]
        self.tt(oeb, oef[0], oef[1], ALU.add)
        prk = PS(7)
        pcs = PS(6)
        for t in range(NT):
            o_ = prk[:, t * 32:(t + 1) * 32]
            self.mm(o_, self.lstrict_b[:], oeb[:, t, :], start=True, stop=(t == 0))
            for t2 in range(t):
                self.mm(o_, self.ones_b[:], oeb[:, t2, :], start=False, stop=(t2 == t - 1))
        for t in range(NT):
            self.mm(pcs[:, 0:32], self.ones_b[:], oeb[:, t, :], start=(t == 0), stop=(t == NT - 1))
        self.tt(slot, prk.rearrange("p (t e) -> p t e", e=32),
                self.carry[:, :].unsqueeze(1).to_broadcast([128, NT, 32]), ALU.add)
        self.tt(self.carry[:], self.carry[:], pcs[:, 0:32], ALU.add)
        iota_bc = self.iota_e[:, :].unsqueeze(1).to_broadcast([128, NT, 32])
        g0 = s * NT
        for k, wk in ((0, wA), (1, wB)):
            self.tt(t32, oef[k], slot, ALU.mult)
            self.reduce(sl, t32, ALU.add)
            self.tt(t32, oef[k], iota_bc, ALU.mult)
            self.reduce(ei, t32, ALU.add)
            self.ts(ge, sl, float(CAP), None, op0=ALU.is_ge)
            self.ts(inv, ge, -1.0, 1.0, op0=ALU.mult, op1=ALU.add)
            self.stt(pos, ei, float(CAP), sl, ALU.mult, ALU.add)
            self.tt(self.wts[:, g0:g0 + NT, k], wk, inv, ALU.mult)
            self.stt(ge, ge, 1.0e6, pos, ALU.mult, ALU.add)
            self.copy(posi[:, :, k], ge)
            self.ts(pos, pos, float(ZR), None, op0=ALU.min)
            self.copy(self.posg[:, g0:g0 + NT, k], pos)
        for t in range(NT):
            for k in range(2):
                self.scatter_rows(self.xg_d, posi[:, t, k:k + 1], zall[:, t, :], NEXP * CAP - 1)

    def phase_moe(self):
        A = self.arena
        PS = self.PS
        T = self.T
        A.release(0)
        NG = CAP // GRP
        NB = GRP // 128
        predf = A.alloc([1, NEXP, NG])
        predi = A.alloc([1, NEXP * NG], I32)
        for g in range(NG):
            self.ts(predf[:, :, g], self.carry[0:1, :], float(g * GRP), None, op0=ALU.is_gt)
        self.copy(predi, predf.rearrange("p e g -> p (e g)"))
        w1b = A.alloc([128, 2, 8, DEXP], BF16)
        w3b = A.alloc([128, 2, 8, DEXP], BF16)
        w2b = A.alloc([128, 2, 4, D], BF16)
        Xg = A.alloc([128, 2, NB, D], BF16)
        XgT = A.alloc([128, 8, GRP], BF16)
        s1 = A.alloc([128, 2, GRP])
        GT = A.alloc([128, 4, GRP], BF16)
        Ysb = A.alloc([128, 2, D], BF16)
        units = [(e, g) for e in range(NEXP) for g in range(NG)]

        def cond(u, fn):
            e, g = units[u]
            if g > 0:
                T.begin_cond(predi[0:1, e * NG + g:e * NG + g + 1])
            fn()
            if g > 0:
                T.end_cond()

        def wloads(e):
            bf = e % 2
            self.dma(w1b[:, bf, :, :], self.w1[e].rearrange("(c p) f -> p c f", p=128), q="pool")
            self.dma(w3b[:, bf, :, :], self.w3[e].rearrange("(c p) f -> p c f", p=128), q="pool")
            self.dma(w2b[:, bf, :, :], self.w2[e].rearrange("(c p) n -> p c n", p=128), q="pool")

        def xloads(u):
            e, g = units[u]
            r = e * CAP + g * GRP
            self.dma(Xg[:, u % 2, :, :], self.xg_d[r:r + GRP, :].rearrange("(b p) n -> p b n", p=128))

        yi = [0]

        def compute(u):
            e, g = units[u]
            bf = e % 2
            xb = u % 2
            for cq in range(2):
                pt = PS(cq).bitcast(BF16)
                for ci in range(4):
                    c = 4 * cq + ci
                    for blk in range(NB):
                        self.tr(pt[:, ci * GRP + blk * 128:ci * GRP + (blk + 1) * 128],
                                Xg[:, xb, blk, c * 128:(c + 1) * 128], self.ident_b[:])
                self.copy(XgT[:, 4 * cq:4 * cq + 4, :], pt[:, 0:4 * GRP].rearrange("p (a n) -> p a n", a=4),
                          eng="dve")
            for fc in range(4):
                ph1 = PS(2 + 2 * (fc % 2))
                ph3 = PS(3 + 2 * (fc % 2))
                fs = slice(fc * 128, (fc + 1) * 128)
                for c in range(8):
                    self.mm(ph1[:, 0:GRP], w1b[:, bf, c, fs], XgT[:, c, :], start=(c == 0), stop=(c == 7))
                for c in range(8):
                    self.mm(ph3[:, 0:GRP], w3b[:, bf, c, fs], XgT[:, c, :], start=(c == 0), stop=(c == 7))
                self.act(s1[:, fc % 2, :], ph1[:, 0:GRP], AF.Silu)
                self.tt(GT[:, fc, :], ph3[:, 0:GRP], s1[:, fc % 2, :], ALU.mult)
            for blk in range(NB):
                yb_ = yi[0] % 2
                yi[0] += 1
                for half in range(2):
                    py = PS(6 + half)
                    for fc in range(4):
                        self.mm(py, GT[:, fc, blk * 128:(blk + 1) * 128], w2b[:, bf, fc, half * 512:(half + 1) * 512],
                                start=(fc == 0), stop=(fc == 3))
                    self.copy(Ysb[:, yb_, half * 512:(half + 1) * 512], py, eng="dve")
                r = e * CAP + g * GRP + blk * 128
                self.dma(self.y_d[r:r + 128, :], Ysb[:, yb_, :])

        wloads(0)
        cond(0, lambda: xloads(0))
        for u, (e, g) in enumerate(units):
            if g == 0 and e + 1 < NEXP:
                wloads(e + 1)
            if u + 1 < len(units):
                cond(u + 1, lambda: xloads(u + 1))
            cond(u, lambda: compute(u))

    def phase_combine(self):
        A = self.arena
        NBUF = 4
        xa = A.alloc([128, NBUF, D])
        ya = A.alloc([128, NBUF, 2, D], BF16)
        ob = A.alloc([128, NBUF, D])
        junk = A.alloc([128, D], BF16)
        ssq = A.alloc([128, NBUF])
        rstd = A.alloc([128, NBUF])
        NT = NSEQ * 16

        def loads(gt):
            b = gt % NBUF
            self.dma(xa[:, b, :], self.x1_d[gt * 128:(gt + 1) * 128, :])
            for k in range(2):
                self.gather_rows(ya[:, b, k, :], self.y_d, self.posg[:, gt, k:k + 1])

        for gt in range(min(2, NT)):
            loads(gt)
        for gt in range(NT):
            b = gt % NBUF
            if gt + 2 < NT:
                loads(gt + 2)
            for k in range(2):
                self.stt(xa[:, b, :], ya[:, b, k, :], self.wts[:, gt, k:k + 1], xa[:, b, :], ALU.mult, ALU.add)
            self.act(junk, xa[:, b, :], AF.Square, accum=ssq[:, b:b + 1])
            self.rsqrt(rstd[:, b:b + 1], ssq[:, b:b + 1], D * EPS)
            self.stt(ob[:, b, :], xa[:, b, :], rstd[:, b:b + 1], self.gfin[:], ALU.mult, ALU.mult)
            self.dma(self.out[gt * 128:(gt + 1) * 128, :], ob[:, b, :])


def _consts():
    inv = (1.0 / (np.float32(10000.0) ** (np.arange(0, 64, 2, dtype=np.float32) / np.float32(64)))).astype(np.float32)
    ang = np.arange(S, dtype=np.float32)[None, :] * inv[:, None]
    cos = np.cos(ang).astype(np.float32)
    sin = np.sin(ang).astype(np.float32)
    rope = np.zeros((128, 2, S), np.float32)
    for c in range(2):
        for f in range(2):
            r = slice(c * 64 + f * 32, c * 64 + f * 32 + 32)
            rope[r, 0, :] = cos
            rope[r, 1, :] = -sin if f == 0 else sin
    ar = np.arange(128)
    oh = np.zeros((128, 4, 128), np.float32)
    sel = np.zeros((128, 64), np.float32)
    for q in range(4):
        for h in range(4):
            oh[32 * q + h, h, :] = 1.0
            sel[32 * q + h, 4 * q + h] = 1.0
    return {
        "c_rope": rope,
        "c_ident": np.eye(128, dtype=np.float32),
        "c_tri": (ar[:, None] <= ar[None, :]).astype(np.float32),
        "c_lstrict": (ar[:, None] < ar[None, :]).astype(np.float32),
        "c_maskneg": np.where(ar[:, None] <= ar[None, :], 0.0, -30000.0).astype(np.float32),
        "c_oh": oh,
        "c_sel": sel,
        "c_iota": np.ascontiguousarray(np.broadcast_to(np.arange(32, dtype=np.float32)[None, :], (128, 32))),
    }


def _in_maps(inputs):
    f32 = lambda a: np.ascontiguousarray(np.asarray(a, np.float32))
    x = f32(inputs["x"])
    wr = np.concatenate([f32(inputs["w_grp"][0]),
                         f32(inputs["w_erouter"][0]).transpose(1, 0, 2).reshape(D, 32)], axis=1)
    rb = np.concatenate([f32(inputs["b_grp"][0]), f32(inputs["b_erouter"][0]).reshape(32)])
    shared = {
        "w_in": f32(inputs["w_in"][0]),
        "w_out": f32(inputs["w_out"][0]),
        "w1": f32(inputs["w1"][0]),
        "w3": f32(inputs["w3"][0]),
        "w2": f32(inputs["w2"][0]),
        "lam_qk": f32(inputs["lam_qk"][0]),
        "subln_g": f32(inputs["subln_g"][0].reshape(128, 1)),
        "g_mix": f32(inputs["g_mix"][0].reshape(8, 128).T),
        "conv_w": f32(inputs["conv_w"][0].T.reshape(8, 128, 4).transpose(1, 0, 2)),
        "conv_b": f32(inputs["conv_b"][0].reshape(8, 128).T),
        "gate_b": f32(inputs["gate_b"][0].T),
        "mhg_bc": f32(np.broadcast_to(inputs["mhnorm_g"][0].reshape(1, 512), (128, 512))),
        "gffn_bc": f32(np.broadcast_to(inputs["g_ffn"][0].reshape(1, D), (128, D))),
        "gfin_bc": f32(np.broadcast_to(np.asarray(inputs["g_final"]).reshape(1, D), (128, D))),
        "gffn_col": f32(inputs["g_ffn"][0].reshape(8, 128).T),
        "wr": f32(wr.reshape(8, 128, 36).transpose(1, 0, 2)),
        "rbias": f32(np.broadcast_to(rb.reshape(1, 36), (128, 36))),
    }
    shared.update(_consts())
    maps = []
    for c in range(NCORES):
        m = {"x": np.ascontiguousarray(x[NSEQ * c:NSEQ * (c + 1)].reshape(TOK, D))}
        m.update(shared)
        maps.append(m)
    return maps


def kernel(**inputs):
    b = Builder()
    nc = b.build()
    maps = _in_maps(inputs)
    res = run_bass_kernel_spmd(nc, maps, core_ids=list(range(NCORES)))
    outs = [np.asarray(r["out"], dtype=np.float32).reshape(NSEQ, S, D) for r in res.results]
    return np.concatenate(outs, axis=0)
```

```python
import math
from contextlib import ExitStack

import numpy as np
import concourse.bass as bass
import concourse.mybir as mybir
from concourse.bass_utils import run_bass_kernel_spmd

F32 = mybir.dt.float32
BF16 = mybir.dt.bfloat16
I32 = mybir.dt.int32
U32 = mybir.dt.uint32
AF = mybir.ActivationFunctionType
ALU = mybir.AluOpType
AX = mybir.AxisListType

NCORES = 8
D = 1024
S = 2048
NSEQ = 2
TOK = NSEQ * S
D_IN = 3592
EPS = 1e-6
LAMBDA_INIT = 0.2
NEXP = 32
DEXP = 512
CAP = 1024
GRP = 256

ENG_NAMES = ("pe", "act", "dve", "pool", "sp")


class _Op:
    __slots__ = ("eng", "fn", "deps", "is_dma", "dsem", "dval", "sig", "sigval", "idx", "waits", "cond")

    def __init__(self, eng, fn, is_dma):
        self.eng = eng
        self.fn = fn
        self.deps = set()
        self.is_dma = is_dma
        self.dsem = None
        self.dval = 0
        self.sig = False
        self.sigval = 0
        self.waits = None
        self.cond = None


_ESZ = {}


def _esize(dt):
    r = _ESZ.get(dt)
    if r is None:
        n = str(dt)
        r = 4 if "32" in n else (2 if "16" in n else (8 if "64" in n else 1))
        _ESZ[dt] = r
    return r


def _region(ap):
    r = _region0(ap)
    e = _esize(ap.dtype)
    lo, hi = r[3] * e, r[4] * e
    if str(ap.space) == "PSUM":
        return (r[0], 0, 128, (lo // 2048) * 2048, ((hi + 2047) // 2048) * 2048)
    return (r[0], r[1], r[2], lo, hi)


def _region0(ap):
    name = ap.tensor.name
    dims = ap.ap
    off = int(ap.offset)
    if str(ap.space) == "DRAM":
        lo = hi = off
        for st, cnt in dims:
            if cnt > 1:
                if st >= 0:
                    hi += st * (cnt - 1)
                else:
                    lo += st * (cnt - 1)
        return (name, 0, 1, lo, hi + 1)
    pst, pcnt = dims[0]
    assert pst > 0, "partition-broadcast SBUF APs not supported by tracker"
    p0 = off // pst
    lo = hi = off % pst
    for st, cnt in dims[1:]:
        if cnt > 1:
            if st >= 0:
                hi += st * (cnt - 1)
            else:
                lo += st * (cnt - 1)
    return (name, p0, p0 + pcnt, lo, hi + 1)


class Tracker:
    def __init__(self, nc, n_dma_sems=20):
        self.nc = nc
        self.ops = []
        self.recs = {}
        self.n_dma_sems = n_dma_sems
        self.dma_rr = {}
        self.dma_last = {}
        self.cur_cond = None
        self.ncond = 0

    def begin_cond(self, pred_ap):
        self.ncond += 1
        self.cur_cond = (self.ncond, pred_ap)

    def end_cond(self):
        self.cur_cond = None

    def _access(self, op, idx, ap, is_write, tag=None):
        name, p0, p1, f0, f1 = _region(ap)
        lst = self.recs.get(name, ())
        keep = []
        for r in lst:
            overlap = not (r[1] <= p0 or p1 <= r[0] or r[3] <= f0 or f1 <= r[2])
            if overlap and r[4] != idx and (r[5] or is_write) and not (tag is not None and r[7] == tag):
                same = (r[6] == op.eng and not op.is_dma and not self.ops[r[4]].is_dma)
                if same:
                    if op.eng != "pe":
                        op.deps.add(r[4])
                else:
                    op.deps.add(r[4])
            if is_write and r[0] >= p0 and r[1] <= p1 and r[2] >= f0 and r[3] <= f1 and r[4] != idx \
                    and not (tag is not None and r[7] == tag):
                continue
            if (not is_write) and (not r[5]) and r[6] == op.eng and not op.is_dma \
                    and r[0] == p0 and r[1] == p1 and r[2] == f0 and r[3] == f1:
                continue
            keep.append(r)
        keep.append([p0, p1, f0, f1, idx, is_write, op.eng, tag])
        self.recs[name] = keep

    def op(self, eng, fn, reads=(), writes=()):
        o = _Op(eng, fn, False)
        idx = len(self.ops)
        o.idx = idx
        o.cond = self.cur_cond
        self.ops.append(o)
        if o.cond is not None:
            self._access(o, idx, o.cond[1], False)
        for ap in writes:
            self._access(o, idx, ap, True)
        for ap in reads:
            self._access(o, idx, ap, False)
        return o

    def dma(self, queue, out, in_, fn=None, extra_reads=(), out_tag=None):
        if fn is None:
            fn = lambda e, out=out, in_=in_: e.dma_start(out=out, in_=in_)
        o = _Op(queue, fn, True)
        idx = len(self.ops)
        o.idx = idx
        o.cond = self.cur_cond
        self.ops.append(o)
        if o.cond is not None:
            self._access(o, idx, o.cond[1], False)
        self._access(o, idx, in_, False)
        for ap in extra_reads:
            self._access(o, idx, ap, False)
        self._access(o, idx, out, True, tag=out_tag)
        k = self.dma_rr.get(queue, 0)
        self.dma_rr[queue] = (k + 1) % self.n_dma_sems
        o.dsem = (queue, k)
        prev = self.dma_last.get((queue, k))
        if prev is not None:
            o.deps.add(prev)
            o.dval = self.ops[prev].dval + 16
        else:
            o.dval = 16
        self.dma_last[(queue, k)] = idx
        return o

    def finalize(self, es):
        nc = self.nc
        ops = self.ops
        for o in ops:
            for d in o.deps:
                if not ops[d].is_dma:
                    ops[d].sig = True
        cnt = {}
        for o in ops:
            if not o.is_dma and o.sig:
                cnt[o.eng] = cnt.get(o.eng, 0) + 1
                o.sigval = cnt[o.eng]
        esem = {e: es.enter_context(nc.semaphore("sem_" + e)) for e in ("pe", "act", "dve", "pool")}
        dsem = {}
        for q in self.dma_rr:
            for k in range(self.n_dma_sems):
                dsem[(q, k)] = es.enter_context(nc.semaphore("dsem_%s_%d" % (q, k)))
        streams = {e: [] for e in ENG_NAMES}
        water = {e: {} for e in ENG_NAMES}
        for o in ops:
            need = {}
            for d in o.deps:
                p = ops[d]
                if p.is_dma:
                    key = ("d",) + p.dsem
                    val = p.dval
                else:
                    key = ("e", p.eng)
                    val = p.sigval
                if val > need.get(key, 0):
                    need[key] = val
            w = []
            wm = water[o.eng]
            for key, val in need.items():
                if wm.get(key, 0) >= val:
                    continue
                wm[key] = val
                sem = esem[key[1]] if key[0] == "e" else dsem[(key[1], key[2])]
                w.append((sem, val))
            o.waits = w
            streams[o.eng].append(o)
        final_waits = [(q, dsem[(q, k)], ops[i].dval) for (q, k), i in self.dma_last.items()]
        self.stats = {e: len(streams[e]) for e in ENG_NAMES}
        block = es.enter_context(nc.Block())

        def emit_op(engine, o, with_waits=True):
            if with_waits:
                for sem, val in o.waits:
                    engine.wait_ge(sem, val)
            ins = o.fn(engine)
            if o.is_dma:
                ins.then_inc(dsem[o.dsem], 16)
            elif o.sig:
                ins.then_inc(esem[o.eng], 1)

        def emit(engine, lst, qname):
            i = 0
            reg = None
            while i < len(lst):
                o = lst[i]
                if o.cond is None:
                    emit_op(engine, o)
                    i += 1
                    continue
                j = i
                while j < len(lst) and lst[j].cond is not None and lst[j].cond[0] == o.cond[0]:
                    j += 1
                run = lst[i:j]
                for sem, val in o.waits:
                    engine.wait_ge(sem, val)
                if reg is None:
                    reg = engine.alloc_register("cnd_" + qname)
                engine.reg_load(reg, o.cond[1])
                with engine.If_ne(reg, 0):
                    for k, r in enumerate(run):
                        emit_op(engine, r, with_waits=(k > 0))
                with engine.Else():
                    nsig = 0
                    first_sig = None
                    for r in run:
                        if r.is_dma:
                            if r.dval > 16:
                                engine.wait_ge(dsem[r.dsem], r.dval - 16)
                            engine.sem_inc(dsem[r.dsem], 16)
                        elif r.sig:
                            nsig += 1
                            if first_sig is None:
                                first_sig = r.sigval
                    if nsig:
                        if first_sig > 1:
                            engine.wait_ge(esem[run[0].eng], first_sig - 1)
                        engine.sem_inc(esem[run[0].eng], nsig)
                i = j
            for q, sem, val in final_waits:
                if q == qname:
                    engine.wait_ge(sem, val)

        @block.tensor
        def _(e):
            emit(e, streams["pe"], "pe")

        @block.scalar
        def _(e):
            emit(e, streams["act"], "act")

        @block.vector
        def _(e):
            emit(e, streams["dve"], "dve")

        @block.gpsimd
        def _(e):
            emit(e, streams["pool"], "pool")

        @block.sync
        def _(e):
            emit(e, streams["sp"], "sp")


ARENA_BYTES = 122880
KSCALE_LN = math.log(128.0 ** -0.5)
ZR = NEXP * CAP


class Arena:
    def __init__(self, b, nbytes):
        self.t = b.sb("AR", [128, nbytes // 2], BF16)
        self.n = nbytes
        self.off = 0
        self.peak = 0

    def mark(self):
        return self.off

    def release(self, m):
        self.off = m

    def alloc(self, shape, dt=F32):
        esz = 2 if dt == BF16 else 4
        nfree = 1
        for d in shape[1:]:
            nfree *= d
        nb = nfree * esz
        start = self.off
        self.off += (nb + 63) // 64 * 64
        self.peak = max(self.peak, self.off)
        assert self.off <= self.n, ("arena overflow", self.off, self.n)
        v = self.t[0:shape[0], start // 2:(start + nb) // 2]
        if dt != BF16:
            v = v.bitcast(dt)
        if len(shape) > 2:
            names = ["a%d" % i for i in range(len(shape) - 1)]
            pat = "p (" + " ".join(names) + ") -> p " + " ".join(names)
            v = v.rearrange(pat, **{n: d for n, d in zip(names, shape[1:])})
        return v


def bcast_free(ap, n):
    return bass.AP(ap.tensor, ap.offset, [[ap.ap[0][0], ap.ap[0][1]], [0, n]])


class StopBuild(Exception):
    pass


class Builder:
    def __init__(self, dbg=None, stop_after=None):
        self.dbg = dbg or ()
        self.stop_after = stop_after
        self.nc = bass.Bass("TRN2", target_bir_lowering=False)
        self.T = Tracker(self.nc)
        self.es = ExitStack()

    def din(self, name, shape, dt=F32):
        return self.nc.dram_tensor(name, list(shape), dt, kind="ExternalInput").ap()

    def dout(self, name, shape, dt=F32):
        return self.nc.dram_tensor(name, list(shape), dt, kind="ExternalOutput").ap()

    def dscratch(self, name, shape, dt=F32):
        return self.nc.dram_tensor(name, list(shape), dt, kind="Internal").ap()

    def sb(self, name, shape, dt=F32):
        return self.es.enter_context(self.nc.sbuf_tensor(name, list(shape), dt))

    def ps(self, name, shape, dt=F32):
        return self.es.enter_context(self.nc.psum_tensor(name, list(shape), dt))

    def mm(self, out, lhsT, rhs, start=True, stop=True):
        self.T.op("pe", lambda e: e.matmul(out, lhsT, rhs, start=start, stop=stop),
                  reads=[lhsT, rhs], writes=[out])

    def tr(self, out, in_, ident):
        self.T.op("pe", lambda e: e.transpose(out, in_, ident), reads=[in_, ident], writes=[out])

    def act(self, out, in_, func, bias=None, scale=None, accum=None):
        reads = [in_]
        kw = {}
        if bias is not None:
            kw["bias"] = bias
            if not isinstance(bias, (int, float)):
                reads.append(bias)
        if scale is not None:
            kw["scale"] = scale
            if not isinstance(scale, (int, float)):
                reads.append(scale)
        writes = [out]
        if accum is not None:
            kw["accum_out"] = accum
            writes.append(accum)
        self.T.op("act", lambda e: e.activation(out, in_, func, **kw), reads=reads, writes=writes)

    def tt(self, out, a, b, op, eng="dve"):
        self.T.op(eng, lambda e: e.tensor_tensor(out, a, b, op), reads=[a, b], writes=[out])

    def ts(self, out, a, s1, s2=None, op0=ALU.mult, op1=None, eng="dve"):
        reads = [a]
        for s in (s1, s2):
            if s is not None and not isinstance(s, (int, float)):
                reads.append(s)
        kw = {}
        if op1 is not None:
            kw["op1"] = op1
        self.T.op(eng, lambda e: e.tensor_scalar(out, a, s1, s2, op0, **kw), reads=reads, writes=[out])

    def stt(self, out, a, scalar, b, op0, op1, eng="dve"):
        reads = [a, b]
        if not isinstance(scalar, (int, float)):
            reads.append(scalar)
        self.T.op(eng, lambda e: e.scalar_tensor_tensor(out, a, scalar, b, op0, op1),
                  reads=reads, writes=[out])

    def copy(self, out, in_, eng="dve"):
        if eng == "act":
            self.T.op("act", lambda e: e.copy(out, in_), reads=[in_], writes=[out])
        else:
            self.T.op(eng, lambda e: e.tensor_copy(out, in_), reads=[in_], writes=[out])

    def rsqrt(self, out, in_, c):
        self.act(out, in_, AF.Ln, bias=float(c))
        self.act(out, out, AF.Exp, scale=-0.5)

    def recip(self, out, in_):
        self.T.op("dve", lambda e: e.reciprocal(out, in_), reads=[in_], writes=[out])

    def reduce(self, out, in_, op, axis=AX.X):
        self.T.op("dve", lambda e: e.tensor_reduce(out, in_, axis, op), reads=[in_], writes=[out])

    def scan(self, out, d0, d1, init, op0, op1, d0_reads=()):
        self.T.op("dve", lambda e: e.tensor_tensor_scan(out, d0, d1, init, op0, op1),
                  reads=list(d0_reads) + [d1], writes=[out])

    def memset(self, ap, val, eng="dve"):
        self.T.op(eng, lambda e: e.memset(ap, val), reads=[], writes=[ap])

    def dma(self, out, in_, q="sp"):
        self.T.dma(q, out, in_)

    def scatter_rows(self, dram, idx_ap, src, bound):
        def fn(e):
            if getattr(self, "_bnd_reg", None) is None:
                self._bnd_reg = e.alloc_register("bnd")
                e.reg_mov(self._bnd_reg, bound)
            return e.indirect_dma_start(
                out=dram, out_offset=bass.IndirectOffsetOnAxis(ap=idx_ap, axis=0), in_=src, in_offset=None,
                bounds_check=self._bnd_reg, oob_is_err=False)
        self.T.dma("pool", dram, src, fn=fn, extra_reads=[idx_ap], out_tag="disjoint_rows")

    def gather_rows(self, dst, dram, idx_ap):
        self.T.dma("pool", dst, dram, fn=lambda e: e.indirect_dma_start(
            out=dst, out_offset=None, in_=dram, in_offset=bass.IndirectOffsetOnAxis(ap=idx_ap, axis=0)),
            extra_reads=[idx_ap])

    def chk(self, name):
        if self.stop_after == name:
            raise StopBuild()

    def PS(self, i):
        return self.PSB[i // 2][:, (i % 2) * 512:(i % 2 + 1) * 512]

    def build(self):
        nc, T = self.nc, self.T
        din, sb = self.din, self.sb
        self.x = din("x", [TOK, D])
        self.w_in = din("w_in", [D, D_IN])
        self.w_out = din("w_out", [D, D])
        self.w1 = din("w1", [NEXP, D, DEXP])
        self.w3 = din("w3", [NEXP, D, DEXP])
        self.w2 = din("w2", [NEXP, DEXP, D])
        c_rope = din("c_rope", [128, 2, S])
        self.c_rope = c_rope
        c_ident = din("c_ident", [128, 128])
        c_tri = din("c_tri", [128, 128])
        c_lstrict = din("c_lstrict", [128, 128])
        c_maskneg = din("c_maskneg", [128, 128])
        c_oh = din("c_oh", [128, 4, 128])
        c_sel = din("c_sel", [128, 64])
        c_iota = din("c_iota", [128, 32])
        lam_d = din("lam_qk", [4, 64])
        subln_d = din("subln_g", [128, 1])
        gmix_d = din("g_mix", [128, 8])
        convw_d = din("conv_w", [128, 8, 4])
        convb_d = din("conv_b", [128, 8])
        gateb_d = din("gate_b", [4, 2])
        mhg_d = din("mhg_bc", [128, 512])
        gffn_d = din("gffn_bc", [128, D])
        gfin_d = din("gfin_bc", [128, D])
        gffnc_d = din("gffn_col", [128, 8])
        wr_d = din("wr", [128, 8, 36])
        rbias_d = din("rbias", [128, 36])
        self.out = self.dout("out", [TOK, D])
        self.x1_d = self.dscratch("x1_d", [TOK, D])
        self.xg_d = self.dscratch("xg_d", [NEXP * CAP, D], BF16)
        self.y_d = self.dscratch("y_d", [NEXP * CAP + 1, D], BF16)

        self.ident_f = sb("ident_f", [128, 128])
        self.ident_b = sb("ident_b", [128, 128], BF16)
        self.tri_b = sb("tri_b", [128, 128], BF16)
        self.lstrict_b = sb("lstrict_b", [128, 128], BF16)
        self.ones_b = sb("ones_b", [128, 128], BF16)
        self.maskneg = sb("maskneg", [128, 128])
        self.OH = sb("OH", [128, 4, 128])
        self.sel = sb("sel", [128, 64])
        self.iota_e = sb("iota_e", [128, 32])
        self.gmix = sb("gmix", [128, 8])
        self.neglam = sb("neglam", [128, 1])
        self.subg = sb("subg", [128, 1])
        self.convw = sb("convw", [128, 8, 4])
        self.convb = sb("convb", [128, 8])
        self.gateb = sb("gateb", [4, 2])
        self.negfb = sb("negfb", [4, 1])
        self.ones4 = sb("ones4", [4, 1])
        self.mhg = sb("mhg", [128, 512])
        self.gffn = sb("gffn", [128, D])
        self.gfin = sb("gfin", [128, D])
        self.gffnc = sb("gffnc", [128, 8])
        self.wr = sb("wr_t", [128, 8, 36])
        self.rbias = sb("rbias_t", [128, 36])
        self.carry = sb("carry", [128, 32])
        self.posg = sb("posg", [128, 32, 2], I32)
        self.wts = sb("wts", [128, 32, 2])
        self.hT = sb("hT", [128, 8, S], BF16)
        self.mixT = sb("mixT", [128, 8, S], BF16)
        self.arena = Arena(self, ARENA_BYTES)
        A = self.arena
        self.PSB = [self.ps("psb%d" % i, [128, 1024]) for i in range(4)]

        m = A.mark()
        tf = A.alloc([128, 128])
        for dst, src in ((self.tri_b, c_tri), (self.lstrict_b, c_lstrict), (self.ident_b, c_ident)):
            self.dma(tf, src)
            self.copy(dst[:], tf)
        self.dma(self.ident_f[:], c_ident)
        self.dma(self.maskneg[:], c_maskneg)
        self.dma(self.OH[:], c_oh)
        self.dma(self.sel[:], c_sel)
        self.dma(self.iota_e[:], c_iota)
        self.dma(self.gmix[:], gmix_d)
        self.dma(self.subg[:], subln_d)
        self.dma(self.convw[:], convw_d)
        self.dma(self.convb[:], convb_d)
        self.dma(self.gateb[:], gateb_d)
        self.dma(self.mhg[:], mhg_d)
        self.dma(self.gffn[:], gffn_d)
        self.dma(self.gfin[:], gfin_d)
        self.dma(self.gffnc[:], gffnc_d)
        self.dma(self.wr[:], wr_d)
        self.dma(self.rbias[:], rbias_d)
        self.memset(self.ones_b[:], 1.0)
        self.memset(self.ones4[:], 1.0)
        self.memset(self.carry[:], 0.0)
        sqD = float(math.sqrt(D))
        self.ts(self.gmix[:], self.gmix[:], sqD, None, op0=ALU.mult)
        self.ts(self.gffn[:], self.gffn[:], sqD, None, op0=ALU.mult)
        self.ts(self.gfin[:], self.gfin[:], sqD, None, op0=ALU.mult)
        self.ts(self.mhg[:], self.mhg[:], float(math.sqrt(128.0)), None, op0=ALU.mult)
        self.ts(self.subg[:], self.subg[:], float((1.0 - LAMBDA_INIT) * math.sqrt(128.0)), None, op0=ALU.mult)
        self.ts(self.negfb[:], self.gateb[:, 1:2], -1.0, None, op0=ALU.mult)
        self.ts(self.gffnc[:], self.gffnc[:], sqD, None, op0=ALU.mult)
        for c in range(8):
            self.ts(self.wr[:, c, :], self.wr[:, c, :], self.gffnc[:, c:c + 1], None, op0=ALU.mult)
        lam_t = A.alloc([128, 256])
        lam_p = A.alloc([128, 2, 64])
        lam_s = A.alloc([128, 2])
        self.dma(lam_t, lam_d.rearrange("a b -> (a b)").partition_broadcast(128))
        lv = lam_t.rearrange("p (a b c) -> p a b c", a=2, b=2)
        self.tt(lam_p, lv[:, :, 0, :], lv[:, :, 1, :], ALU.mult)
        self.reduce(lam_s, lam_p, ALU.add)
        self.act(lam_s, lam_s, AF.Exp)
        self.tt(self.neglam[:], lam_s[:, 1:2], lam_s[:, 0:1], ALU.subtract)
        self.ts(self.neglam[:], self.neglam[:], -LAMBDA_INIT, None, op0=ALU.add)
        zrow = A.alloc([1, D], BF16)
        self.memset(zrow, 0.0)
        self.dma(self.y_d[ZR:ZR + 1, :], zrow)
        A.release(m)

        self.w_in_v = self.w_in.rearrange("(c p) n -> p c n", p=128)
        done = False
        try:
            for s in range(NSEQ):
                m = A.mark()
                self.phase_norm(s)
                self.phase_attn(s)
                A.release(m)
                self.chk("ATT")
                self.phase_mlstm(s)
                A.release(m)
                self.chk("ML")
                self.phase_outproj(s)
                A.release(m)
                self.chk("C")
        except StopBuild:
            done = True
        if not done:
            self.phase_moe()
            A.release(0)
            self.phase_combine()

        dbg = self.dbg
        if "mixT" in dbg:
            d = self.dout("dbg_mixT", [128, 8, S], BF16)
            self.dma(d, self.mixT[:])
        if "route" in dbg:
            d = self.dout("dbg_posg", [128, 32, 2], I32)
            self.dma(d, self.posg[:])
            d = self.dout("dbg_wts", [128, 32, 2])
            self.dma(d, self.wts[:])
        if "x1" in dbg:
            d = self.dout("dbg_x1", [S, D])
            self.dma(d, self.x1_d[0:S, :])
        if done:
            zt = self.sb("zt_dummy", [128, D])
            self.memset(zt[:], 0.0)
            self.dma(self.out[0:128, :], zt[:])
        T.finalize(self.es)
        return nc

    def phase_norm(self, s):
        A = self.arena
        xt = A.alloc([128, 2, D])
        xsq = A.alloc([128, D], BF16)
        xs = A.alloc([128, 2, D], BF16)
        ssq = A.alloc([128, 2])
        rstd = A.alloc([128, 2])
        hT = self.hT
        for t in range(16):
            b = t % 2
            r0 = s * S + t * 128
            self.dma(xt[:, b, :], self.x[r0:r0 + 128, :])
            self.act(xsq, xt[:, b, :], AF.Square, accum=ssq[:, b:b + 1])
            self.rsqrt(rstd[:, b:b + 1], ssq[:, b:b + 1], D * EPS)
            self.ts(xs[:, b, :], xt[:, b, :], rstd[:, b:b + 1], None, op0=ALU.mult)
            pst = self.PS(b).bitcast(BF16)
            for c in range(8):
                self.tr(pst[:, c * 128:(c + 1) * 128], xs[:, b, c * 128:(c + 1) * 128], self.ident_b[:])
            self.tt(hT[:, :, t * 128:(t + 1) * 128], pst.rearrange("p (c t) -> p c t", c=8),
                    self.gmix[:, :].unsqueeze(2).to_broadcast([128, 8, 128]), ALU.mult)

    def phase_attn(self, s):
        A = self.arena
        hT = self.hT
        rope_t = A.alloc([128, 2, S])
        wv = A.alloc([128, 8, 512], BF16)
        wqk = A.alloc([128, 2, 8, 128], BF16)
        wsw = A.alloc([128, 2, 8, 128], BF16)
        QT = A.alloc([128, S], BF16)
        KT = A.alloc([128, S], BF16)
        Vt = A.alloc([128, 16, 512], BF16)
        PT = A.alloc([128, 4, 512], BF16)
        rp_a = A.alloc([128, 512])
        rp_b = A.alloc([128, 512])
        tmp = (A.alloc([128, 512]), A.alloc([128, 512]), A.alloc([128, 512]), A.alloc([128, 512], BF16),
               A.alloc([128, 512]))
        self.dma(rope_t, self.c_rope)
        self.dma(wv, self.w_in_v[:, :, 1024:1536], q="pool")
        for t in range(16):
            pb = self.PS(2 + t % 2)
            for c in range(8):
                self.mm(pb, hT[:, c, t * 128:(t + 1) * 128], wv[:, c, :], start=(c == 0), stop=(c == 7))
            self.copy(Vt[:, t, :], pb, eng=("act" if t % 2 == 0 else "dve"))
        for h in range(4):
            self.dma(wqk[:, 0, :, :], self.w_in_v[:, :, h * 128:(h + 1) * 128], q="pool")
            self.dma(wqk[:, 1, :, :], self.w_in_v[:, :, 512 + h * 128:512 + (h + 1) * 128], q="pool")
            for j in range(2):
                src = wqk[:, j, :, :].rearrange("p k (c f i) -> p k c f i", c=2, f=2)
                dst = wsw[:, j, :, :].rearrange("p k (c f i) -> p k c f i", c=2, f=2)
                for f in range(2):
                    self.copy(dst[:, :, :, f, :], src[:, :, :, 1 - f, :], eng="pool")
            for j, dstT in ((0, QT), (1, KT)):
                for n in range(4):
                    p1 = self.PS(4 + (n % 2) * 2)
                    p2 = self.PS(5 + (n % 2) * 2)
                    for c in range(8):
                        self.mm(p1, wqk[:, j, c, :], hT[:, c, n * 512:(n + 1) * 512], start=(c == 0), stop=(c == 7))
                    for c in range(8):
                        self.mm(p2, wsw[:, j, c, :], hT[:, c, n * 512:(n + 1) * 512], start=(c == 0), stop=(c == 7))
                    self.tt(rp_a, p1, rope_t[:, 0, n * 512:(n + 1) * 512], ALU.mult)
                    self.tt(rp_b, p2, rope_t[:, 1, n * 512:(n + 1) * 512], ALU.mult)
                    self.tt(dstT[:, n * 512:(n + 1) * 512], rp_a, rp_b, ALU.add)
            self.attention(h, QT, KT, Vt, PT, tmp)

    def attention(self, h, QT, KT, Vt, PT, tmp):
        e_r, e_a, e_b, e_sq, e_rs = tmp
        PS = self.PS
        SB = [PS(0), PS(1), PS(2)]
        OT = [PS(3), PS(4)]
        SS = [PS(5), PS(6)]
        SQ = PS(7)
        mixT = self.mixT
        ones_b, tri_b, neglam, subg = self.ones_b, self.tri_b, self.neglam, self.subg
        scale = 1.0 / 8.0
        steps = []
        for qc in range(4):
            last = 4 * qc + 3
            for kt in range(last + 1):
                if kt < 4 * qc:
                    q0, n = qc * 512, 512
                else:
                    j = kt - 4 * qc
                    q0, n = qc * 512 + j * 128, 512 - j * 128
                steps.append((qc, kt, q0, n, kt == 0, kt == last))

        def qk_exp(i):
            qc, kt, q0, n, first, lastf = steps[i]
            for c in range(2):
                sbk = SB[(2 * i + c) % 3]
                pslot = (2 * i + c) % 4
                self.mm(sbk[:, 0:n], KT[c * 64:(c + 1) * 64, kt * 128:(kt + 1) * 128],
                        QT[c * 64:(c + 1) * 64, q0:q0 + n])
                self.act(PT[:, pslot, 0:n], sbk[:, 0:n], AF.Exp, scale=scale)
                if kt >= 4 * qc:
                    self.tt(PT[:, pslot, 0:128], PT[:, pslot, 0:128], tri_b[:], ALU.mult)

        def pv(i):
            qc, kt, q0, n, first, lastf = steps[i]
            off = q0 - qc * 512
            for c in range(2):
                pslot = (2 * i + c) % 4
                self.mm(OT[c][:, off:off + n], Vt[:, kt, h * 128:(h + 1) * 128], PT[:, pslot, 0:n],
                        start=first, stop=lastf)
                self.mm(SS[c][:, off:off + n], ones_b[:], PT[:, pslot, 0:n], start=first, stop=lastf)

        def epi1(qc):
            self.act(e_r, SS[0], AF.Ln)
            self.act(e_r, e_r, AF.Exp, scale=-1.0)
            self.act(e_rs, SS[1], AF.Ln)
            self.act(e_rs, e_rs, AF.Exp, scale=-1.0)
            self.tt(e_a, OT[0], e_r, ALU.mult)
            self.tt(e_b, OT[1], e_rs, ALU.mult)
            self.stt(e_a, e_b, neglam[:, 0:1], e_a, ALU.mult, ALU.add)
            self.tt(e_sq, e_a, e_a, ALU.mult)

        def epi2(qc):
            self.mm(SQ, ones_b[:], e_sq)
            self.rsqrt(e_rs, SQ, 128 * EPS)
            self.stt(mixT[:, h, qc * 512:(qc + 1) * 512], e_a, subg[:, 0:1], e_rs, ALU.mult, ALU.mult)

        qk_exp(0)
        pending = None
        for i in range(len(steps)):
            if i + 1 < len(steps):
                qk_exp(i + 1)
            pv(i)
            if pending is not None:
                epi2(pending)
                pending = None
            if steps[i][5]:
                epi1(steps[i][0])
                pending = steps[i][0]
        if pending is not None:
            epi2(pending)

    def phase_mlstm(self, s):
        A = self.arena
        PS = self.PS
        hT, mixT = self.hT, self.mixT
        w_in_v = self.w_in_v
        Vm = A.alloc([128, 16, 4, 129], BF16)
        sgo = A.alloc([128, 16, 512], BF16)
        qmT = A.alloc([128, 4, S], BF16)
        kmT = A.alloc([128, 4, S], BF16)
        RQa = A.alloc([128, S])
        cols = A.alloc([128, 16, 16])
        screp = A.alloc([128, 4, 16])
        mask4 = A.alloc([128, 4, 128])
        wqm = A.alloc([128, 2, 8, 128], BF16)
        wg = A.alloc([128, 8, 8], BF16)
        Mj = A.alloc([4, 17])
        sc = A.alloc([4, 64])
        m1 = A.mark()
        wvo = A.alloc([128, 8, 1024], BF16)
        self.dma(wvo, w_in_v[:, :, 2560:3584], q="pool")
        self.memset(Vm[:, :, :, 128:129], 1.0)
        for h in range(4):
            self.ts(mask4[:, h, :], self.tri_b[:], 1.0, None, op0=ALU.mult)
        for t in range(16):
            pa = PS(0 + 2 * (t % 2))
            pb = PS(1 + 2 * (t % 2))
            for c in range(8):
                self.mm(pa, hT[:, c, t * 128:(t + 1) * 128], wvo[:, c, 0:512], start=(c == 0), stop=(c == 7))
            for c in range(8):
                self.mm(pb, hT[:, c, t * 128:(t + 1) * 128], wvo[:, c, 512:1024], start=(c == 0), stop=(c == 7))
            self.copy(Vm[:, t, :, 0:128], pa.rearrange("p (h d) -> p h d", h=4), eng="dve")
            self.act(sgo[:, t, :], pb, AF.Sigmoid)
        A.release(m1)
        self.chk("ML_a")
        T0 = A.alloc([4, S])
        T1 = A.alloc([4, S])
        T2 = A.alloc([4, S])
        T3 = A.alloc([4, S])
        E = T1
        ones_row = bcast_free(self.ones4[:, 0:1], S)
        self.dma(wg, w_in_v[:, :, 3584:3592], q="pool")
        self.memset(RQa, 0.0)
        self.memset(sc, 0.0)
        for n in range(4):
            pi = PS(4 + 2 * (n % 2))
            pf = PS(5 + 2 * (n % 2))
            ns = slice(n * 512, (n + 1) * 512)
            for c in range(8):
                self.mm(pi[0:4, :], wg[:, c, 0:4], hT[:, c, ns], start=(c == 0), stop=(c == 7))
            for c in range(8):
                self.mm(pf[0:4, :], wg[:, c, 4:8], hT[:, c, ns], start=(c == 0), stop=(c == 7))
            self.ts(T0[:, ns], pi[0:4, :], self.gateb[:, 0:1], None, op0=ALU.add)
            self.act(E[:, ns], pf[0:4, :], AF.Exp, bias=self.negfb[:, 0:1], scale=-1.0)
        self.act(E, E, AF.Ln, bias=1.0)
        self.scan(T2, ones_row, E, 0.0, ALU.mult, ALU.add, d0_reads=[self.ones4[:]])
        self.tt(T0, T0, T2, ALU.add)
        self.scan(T3, ones_row, T0, 0.0, ALU.mult, ALU.max, d0_reads=[self.ones4[:]])
        self.tt(T1, T2, T3, ALU.subtract)
        self.act(T1, T1, AF.Exp)
        self.copy(RQa[96:100, :], T1)
        self.memset(Mj[:, 0:1], 0.0)
        T3v = T3.rearrange("p (j l) -> p j l", l=128)
        T0v = T0.rearrange("p (j l) -> p j l", l=128)
        T1v = T1.rearrange("p (j l) -> p j l", l=128)
        self.copy(Mj[:, 1:17], T3v[:, :, 127])
        self.tt(T1v, T3v, Mj[:, 0:16].unsqueeze(2).to_broadcast([4, 16, 128]), ALU.subtract)
        self.act(T1, T1, AF.Exp, scale=-1.0)
        self.copy(RQa[64:68, :], T1)
        self.tt(T1v, T3v, Mj[:, 1:17].unsqueeze(2).to_broadcast([4, 16, 128]), ALU.subtract)
        self.act(T1, T1, AF.Exp, scale=-1.0)
        self.copy(RQa[0:4, :], T1)
        self.tt(T1v, T0v, Mj[:, 1:17].unsqueeze(2).to_broadcast([4, 16, 128]), ALU.subtract)
        self.act(T1, T1, AF.Exp, bias=float(KSCALE_LN))
        self.copy(RQa[32:36, :], T1)
        self.tt(sc[:, 0:16], Mj[:, 0:16], Mj[:, 1:17], ALU.subtract)
        self.act(sc[:, 0:16], sc[:, 0:16], AF.Exp)
        self.chk("ML_b1")
        pc = self.PSB[0][:, :]
        for j in range(16):
            self.mm(pc[:, j * 64:(j + 1) * 64], RQa[:, j * 128:(j + 1) * 128], self.sel[:, :])
        self.copy(cols, pc.rearrange("p (j n) -> p j n", n=64)[:, :, 0:16])
        pr = PS(2)
        for h in range(4):
            self.mm(pr[:, h * 64:(h + 1) * 64], self.OH[0:4, h, :], sc[:, :])
        self.copy(screp, pr[:, 0:256].rearrange("p (h n) -> p h n", n=64)[:, :, 0:16])
        A.release(m1)
        self.chk("ML_b")
        xc = A.alloc([128, 2, S + 4])
        yb = A.alloc([128, 2, S])
        self.memset(xc[:, :, 0:3], 0.0)
        for h in range(4):
            self.dma(wqm[:, 0, :, :], w_in_v[:, :, 1536 + h * 128:1536 + (h + 1) * 128], q="pool")
            self.dma(wqm[:, 1, :, :], w_in_v[:, :, 2048 + h * 128:2048 + (h + 1) * 128], q="pool")
            for j in range(2):
                for n in range(4):
                    p = PS(4 + (2 * j + n) % 4)
                    for c in range(8):
                        self.mm(p, wqm[:, j, c, :], hT[:, c, n * 512:(n + 1) * 512], start=(c == 0), stop=(c == 7))
                    self.copy(xc[:, j, 3 + n * 512:3 + (n + 1) * 512], p, eng="act")
                ct = h + 4 * j
                self.ts(yb[:, j, :], xc[:, j, 3:3 + S], self.convw[:, ct, 3:4], self.convb[:, ct:ct + 1],
                        op0=ALU.mult, op1=ALU.add)
                for tap in (2, 1, 0):
                    self.stt(yb[:, j, :], xc[:, j, tap:tap + S], self.convw[:, ct, tap:tap + 1], yb[:, j, :],
                             ALU.mult, ALU.add)
                self.act((qmT if j == 0 else kmT)[:, h, :], yb[:, j, :], AF.Silu)
        A.release(m1)
        self.chk("ML_c")
        SmT = A.alloc([128, 512], BF16)
        num = A.alloc([128, 4, 129])
        t2 = A.alloc([128, 4, 129])
        hg = A.alloc([128, 4, 128])
        sq = A.alloc([128, 4, 128])
        ss = A.alloc([128, 4])
        rs = A.alloc([128, 4])
        den = A.alloc([128, 4])
        omix = A.alloc([128, 4, 128], BF16)
        wsv = A.alloc([128, 2, 4, 129], BF16)
        CT = A.alloc([128, 4, 129])
        CTb = A.alloc([128, 4, 129], BF16)
        ktok = A.alloc([128, 2, 4, 128], BF16)
        ST, TR = PS(0), PS(1)
        U, N1, N2 = self.PSB[1][:, :], self.PSB[2][:, :], self.PSB[3][:, :]
        trp = TR.bitcast(BF16)

        def v4(ps2):
            return ps2.rearrange("p (b r) -> p b r", b=2)[:, :, 0:258].rearrange("p b (g d) -> p b g d", g=2)

        def hv(ps2, h):
            o = (h // 2) * 512 + (h % 2) * 129
            return ps2[:, o:o + 129]

        N14, N24, U4 = v4(N1), v4(N2), v4(U)
        CT4 = CT.rearrange("p (b g) d -> p b g d", b=2)
        num4 = num.rearrange("p (b g) d -> p b g d", b=2)
        t24 = t2.rearrange("p (b g) d -> p b g d", b=2)
        ident_b = self.ident_b

        def col4(j, q):
            return cols[:, j, 4 * q:4 * q + 4].rearrange("p (b g) -> p b g", b=2).unsqueeze(3).to_broadcast(
                [128, 2, 2, 129])

        for j in range(16):
            js = slice(j * 128, (j + 1) * 128)
            kb = j % 2
            for h in range(4):
                self.tr(trp[:, h * 128:(h + 1) * 128], kmT[:, h, js], ident_b[:])
            self.copy(ktok[:, kb, :, :], trp[:, 0:512].rearrange("p (h t) -> p h t", h=4), eng="act")
            for h in range(4):
                self.mm(ST[:, h * 128:(h + 1) * 128], kmT[:, h, js], qmT[:, h, js])
            self.tt(wsv[:, kb, :, :], Vm[:, j, :, :], cols[:, j, 4:8].unsqueeze(2).to_broadcast([128, 4, 129]),
                    ALU.mult, eng="pool")
            self.tt(SmT, ST, mask4.rearrange("p h l -> p (h l)"), ALU.mult)
            for h in range(4):
                self.mm(hv(N1, h), SmT[:, h * 128:(h + 1) * 128], wsv[:, kb, h, :])
            if j > 0:
                for h in range(4):
                    self.mm(hv(N2, h), qmT[:, h, js], CTb[:, h, :])
            self.tt(num4, N14, col4(j, 0), ALU.mult)
            if j > 0:
                self.tt(t24, N24, col4(j, 2), ALU.mult)
                self.tt(num, num, t2, ALU.add)
            self.stt(den, num[:, :, 128], -1.0, num[:, :, 128], ALU.mult, ALU.max)
            self.tt(den, den, cols[:, j, 12:16], ALU.max)
            self.recip(den, den)
            self.tt(hg, num[:, :, 0:128], den.unsqueeze(2).to_broadcast([128, 4, 128]), ALU.mult)
            self.tt(hg, hg, sgo[:, j, :].rearrange("p (h d) -> p h d", h=4), ALU.mult)
            self.tt(sq, hg, hg, ALU.mult)
            self.reduce(ss, sq, ALU.add)
            self.rsqrt(rs, ss, 128 * EPS)
            self.tt(hg, hg, rs.unsqueeze(2).to_broadcast([128, 4, 128]), ALU.mult)
            self.tt(omix, hg, self.mhg[:, :].rearrange("p (h d) -> p h d", h=4), ALU.mult)
            for h in range(4):
                self.tr(trp[:, 512 + h * 128:512 + (h + 1) * 128], omix[:, h, :], ident_b[:])
            self.copy(mixT[:, 4:8, js], trp[:, 512:1024].rearrange("p (h t) -> p h t", h=4), eng="act")
            if j < 15:
                for h in range(4):
                    self.mm(hv(U, h), ktok[:, kb, h, :], wsv[:, kb, h, :])
                if j == 0:
                    self.copy(CT4, U4)
                else:
                    self.tt(CT, CT, screp[:, :, j:j + 1].to_broadcast([128, 4, 129]), ALU.mult)
                    self.tt(CT4, CT4, U4, ALU.add)
                self.copy(CTb, CT, eng="act")

    def phase_outproj(self, s):
        A = self.arena
        PS = self.PS
        mixT = self.mixT
        NT = 16
        wout = A.alloc([128, 8, D], BF16)
        xt = A.alloc([128, 2, D])
        x1 = A.alloc([128, 2, D])
        zall = A.alloc([128, NT, D], BF16)
        junk = A.alloc([128, D], BF16)
        x1T = A.alloc([128, 8, 128])
        ssq = A.alloc([128, 2])
        rstd = A.alloc([128, 2])
        Lall = A.alloc([128, NT, 36])
        self.dma(wout, self.w_out.rearrange("(c p) n -> p c n", p=128), q="pool")
        def outproj_tile(t):
            b = t % 2
            r0 = s * S + t * 128
            self.dma(xt[:, b, :], self.x[r0:r0 + 128, :])
            for half in range(2):
                p = PS(half + 2 * (t % 2))
                hs = slice(half * 512, (half + 1) * 512)
                for c in range(8):
                    self.mm(p, mixT[:, c, t * 128:(t + 1) * 128], wout[:, c, hs], start=(c == 0), stop=(c == 7))
                self.tt(x1[:, b, hs], p, xt[:, b, hs], ALU.add)
            self.act(junk, x1[:, b, :], AF.Square, accum=ssq[:, b:b + 1])
            self.rsqrt(rstd[:, b:b + 1], ssq[:, b:b + 1], D * EPS)
            self.dma(self.x1_d[r0:r0 + 128, :], x1[:, b, :], q="act")
            self.stt(zall[:, t, :], x1[:, b, :], rstd[:, b:b + 1], self.gffn[:], ALU.mult, ALU.mult)

        def router_tile(t):
            b = t % 2
            pT = self.PSB[2][:, :]
            for c in range(8):
                self.tr(pT[:, c * 128:(c + 1) * 128], x1[:, b, c * 128:(c + 1) * 128], self.ident_f[:])
            x1Tf = x1T.rearrange("p c t -> p (c t)")
            self.copy(x1Tf[:, 0:512], pT[:, 0:512], eng="act")
            self.copy(x1Tf[:, 512:1024], pT[:, 512:1024], eng="dve")
            pl = PS(6)
            for c in range(8):
                self.mm(pl[:, 0:36], x1T[:, c, :], self.wr[:, c, :], start=(c == 0), stop=(c == 7))
            self.stt(Lall[:, t, :], pl[:, 0:36], rstd[:, b:b + 1], self.rbias[:], ALU.mult, ALU.add)

        outproj_tile(0)
        for t in range(NT):
            if t + 1 < NT:
                outproj_tile(t + 1)
            router_tile(t)
        def col():
            return A.alloc([128, NT])
        gmax, gsum, gw, m1_, m2_, dd, e2, rr, wA, wB = [col() for _ in range(10)]
        ohg = A.alloc([128, NT, 4])
        gexp = A.alloc([128, NT, 4])
        t48 = A.alloc([128, NT, 4, 8])
        esel = A.alloc([128, NT, 8])
        esel2 = A.alloc([128, NT, 8])
        oh1 = A.alloc([128, NT, 8])
        oh2 = A.alloc([128, NT, 8])
        oe = [A.alloc([128, NT, 4, 8]), A.alloc([128, NT, 4, 8])]
        oeb = A.alloc([128, NT, 32], BF16)
        slot = A.alloc([128, NT, 32])
        t32 = A.alloc([128, NT, 32])
        sl, ei, ge, inv, pos = [col() for _ in range(5)]
        posi = A.alloc([128, NT, 2], I32)
        Lg4 = Lall[:, :, 0:4]

        def bc(c_, n):
            return c_.unsqueeze(2).to_broadcast([128, NT, n])

        self.reduce(gmax, Lg4, ALU.max)
        self.tt(ohg, Lg4, bc(gmax, 4), ALU.is_equal)
        self.tt(gexp, Lg4, bc(gmax, 4), ALU.subtract)
        self.act(gexp, gexp, AF.Exp)
        self.reduce(gsum, gexp, ALU.add)
        self.recip(gw, gsum)
        self.tt(t48, Lall[:, :, 4:36].rearrange("p t (g e) -> p t g e", g=4),
                ohg.unsqueeze(3).to_broadcast([128, NT, 4, 8]), ALU.mult)
        self.reduce(esel, t48.rearrange("p t g e -> p t e g"), ALU.add)
        self.reduce(m1_, esel, ALU.max)
        self.tt(oh1, esel, bc(m1_, 8), ALU.is_equal)
        self.stt(esel2, oh1, -1e30, esel, ALU.mult, ALU.add)
        self.reduce(m2_, esel2, ALU.max)
        self.tt(oh2, esel2, bc(m2_, 8), ALU.is_equal)
        self.tt(dd, m2_, m1_, ALU.subtract)
        self.act(e2, dd, AF.Exp)
        self.ts(rr, e2, 1.0, None, op0=ALU.add)
        self.recip(rr, rr)
        self.tt(wA, rr, gw, ALU.mult)
        self.tt(wB, e2, wA, ALU.mult)
        for k, ohk in ((0, oh1), (1, oh2)):
            self.copy(oe[k], ohk.unsqueeze(2).to_broadcast([128, NT, 4, 8]))
            self.tt(oe[k], oe[k], ohg.unsqueeze(3).to_broadcast([128, NT, 4, 8]), ALU.mult)
        oef = [o_.rearrange("p t g e -> p t (g e)") for o_ in oe]
        self.tt(oeb, oef[0], oef[1], ALU.add)
        prk = PS(7)
        pcs = PS(6)
        for t in range(NT):
            o_ = prk[:, t * 32:(t + 1) * 32]
            self.mm(o_, self.lstrict_b[:], oeb[:, t, :], start=True, stop=(t == 0))
            for t2 in range(t):
                self.mm(o_, self.ones_b[:], oeb[:, t2, :], start=False, stop=(t2 == t - 1))
        for t in range(NT):
            self.mm(pcs[:, 0:32], self.ones_b[:], oeb[:, t, :], start=(t == 0), stop=(t == NT - 1))
        self.tt(slot, prk.rearrange("p (t e) -> p t e", e=32),
                self.carry[:, :].unsqueeze(1).to_broadcast([128, NT, 32]), ALU.add)
        self.tt(self.carry[:], self.carry[:], pcs[:, 0:32], ALU.add)
        iota_bc = self.iota_e[:, :].unsqueeze(1).to_broadcast([128, NT, 32])
        g0 = s * NT
        for k, wk in ((0, wA), (1, wB)):
            self.tt(t32, oef[k], slot, ALU.mult)
            self.reduce(sl, t32, ALU.add)
            self.tt(t32, oef[k], iota_bc, ALU.mult)
            self.reduce(ei, t32, ALU.add)
            self.ts(ge, sl, float(CAP), None, op0=ALU.is_ge)
            self.ts(inv, ge, -1.0, 1.0, op0=ALU.mult, op1=ALU.add)
            self.stt(pos, ei, float(CAP), sl, ALU.mult, ALU.add)
            self.tt(self.wts[:, g0:g0 + NT, k], wk, inv, ALU.mult)
            self.stt(ge, ge, 1.0e6, pos, ALU.mult, ALU.add)
            self.copy(posi[:, :, k], ge)
            self.ts(pos, pos, float(ZR), None, op0=ALU.min)
            self.copy(self.posg[:, g0:g0 + NT, k], pos)
        for t in range(NT):
            for k in range(2):
                self.scatter_rows(self.xg_d, posi[:, t, k:k + 1], zall[:, t, :], NEXP * CAP - 1)

    def phase_moe(self):
        A = self.arena
        PS = self.PS
        T = self.T
        A.release(0)
        NG = CAP // GRP
        NB = GRP // 128
        predf = A.alloc([1, NEXP, NG])
        predi = A.alloc([1, NEXP * NG], I32)
        for g in range(NG):
            self.ts(predf[:, :, g], self.carry[0:1, :], float(g * GRP), None, op0=ALU.is_gt)
        self.copy(predi, predf.rearrange("p e g -> p (e g)"))
        w1b = A.alloc([128, 2, 8, DEXP], BF16)
        w3b = A.alloc([128, 2, 8, DEXP], BF16)
        w2b = A.alloc([128, 2, 4, D], BF16)
        Xg = A.alloc([128, 2, NB, D], BF16)
        XgT = A.alloc([128, 8, GRP], BF16)
        s1 = A.alloc([128, 2, GRP])
        GT = A.alloc([128, 4, GRP], BF16)
        Ysb = A.alloc([128, 2, D], BF16)
        units = [(e, g) for e in range(NEXP) for g in range(NG)]

        def cond(u, fn):
            e, g = units[u]
            if g > 0:
                T.begin_cond(predi[0:1, e * NG + g:e * NG + g + 1])
            fn()
            if g > 0:
                T.end_cond()

        def wloads(e):
            bf = e % 2
            self.dma(w1b[:, bf, :, :], self.w1[e].rearrange("(c p) f -> p c f", p=128), q="pool")
            self.dma(w3b[:, bf, :, :], self.w3[e].rearrange("(c p) f -> p c f", p=128), q="pool")
            self.dma(w2b[:, bf, :, :], self.w2[e].rearrange("(c p) n -> p c n", p=128), q="pool")

        def xloads(u):
            e, g = units[u]
            r = e * CAP + g * GRP
            self.dma(Xg[:, u % 2, :, :], self.xg_d[r:r + GRP, :].rearrange("(b p) n -> p b n", p=128))

        yi = [0]

        def compute(u):
            e, g = units[u]
            bf = e % 2
            xb = u % 2
            for cq in range(2):
                pt = PS(cq).bitcast(BF16)
                for ci in range(4):
                    c = 4 * cq + ci
                    for blk in range(NB):
                        self.tr(pt[:, ci * GRP + blk * 128:ci * GRP + (blk + 1) * 128],
                                Xg[:, xb, blk, c * 128:(c + 1) * 128], self.ident_b[:])
                self.copy(XgT[:, 4 * cq:4 * cq + 4, :], pt[:, 0:4 * GRP].rearrange("p (a n) -> p a n", a=4),
                          eng="dve")
            for fc in range(4):
                ph1 = PS(2 + 2 * (fc % 2))
                ph3 = PS(3 + 2 * (fc % 2))
                fs = slice(fc * 128, (fc + 1) * 128)
                for c in range(8):
                    self.mm(ph1[:, 0:GRP], w1b[:, bf, c, fs], XgT[:, c, :], start=(c == 0), stop=(c == 7))
                for c in range(8):
                    self.mm(ph3[:, 0:GRP], w3b[:, bf, c, fs], XgT[:, c, :], start=(c == 0), stop=(c == 7))
                self.act(s1[:, fc % 2, :], ph1[:, 0:GRP], AF.Silu)
                self.tt(GT[:, fc, :], ph3[:, 0:GRP], s1[:, fc % 2, :], ALU.mult)
            for blk in range(NB):
                yb_ = yi[0] % 2
                yi[0] += 1
                for half in range(2):
                    py = PS(6 + half)
                    for fc in range(4):
                        self.mm(py, GT[:, fc, blk * 128:(blk + 1) * 128], w2b[:, bf, fc, half * 512:(half + 1) * 512],
                                start=(fc == 0), stop=(fc == 3))
                    self.copy(Ysb[:, yb_, half * 512:(half + 1) * 512], py, eng="dve")
                r = e * CAP + g * GRP + blk * 128
                self.dma(self.y_d[r:r + 128, :], Ysb[:, yb_, :])

        wloads(0)
        cond(0, lambda: xloads(0))
        for u, (e, g) in enumerate(units):
            if g == 0 and e + 1 < NEXP:
                wloads(e + 1)
            if u + 1 < len(units):
                cond(u + 1, lambda: xloads(u + 1))
            cond(u, lambda: compute(u))

    def phase_combine(self):
        A = self.arena
        NBUF = 4
        xa = A.alloc([128, NBUF, D])
        ya = A.alloc([128, NBUF, 2, D], BF16)
        ob = A.alloc([128, NBUF, D])
        junk = A.alloc([128, D], BF16)
        ssq = A.alloc([128, NBUF])
        rstd = A.alloc([128, NBUF])
        NT = NSEQ * 16

        def loads(gt):
            b = gt % NBUF
            self.dma(xa[:, b, :], self.x1_d[gt * 128:(gt + 1) * 128, :])
            for k in range(2):
                self.gather_rows(ya[:, b, k, :], self.y_d, self.posg[:, gt, k:k + 1])

        for gt in range(min(2, NT)):
            loads(gt)
        for gt in range(NT):
            b = gt % NBUF
            if gt + 2 < NT:
                loads(gt + 2)
            for k in range(2):
                self.stt(xa[:, b, :], ya[:, b, k, :], self.wts[:, gt, k:k + 1], xa[:, b, :], ALU.mult, ALU.add)
            self.act(junk, xa[:, b, :], AF.Square, accum=ssq[:, b:b + 1])
            self.rsqrt(rstd[:, b:b + 1], ssq[:, b:b + 1], D * EPS)
            self.stt(ob[:, b, :], xa[:, b, :], rstd[:, b:b + 1], self.gfin[:], ALU.mult, ALU.mult)
            self.dma(self.out[gt * 128:(gt + 1) * 128, :], ob[:, b, :], q="act")


def _consts():
    inv = (1.0 / (np.float32(10000.0) ** (np.arange(0, 64, 2, dtype=np.float32) / np.float32(64)))).astype(np.float32)
    ang = np.arange(S, dtype=np.float32)[None, :] * inv[:, None]
    cos = np.cos(ang).astype(np.float32)
    sin = np.sin(ang).astype(np.float32)
    rope = np.zeros((128, 2, S), np.float32)
    for c in range(2):
        for f in range(2):
            r = slice(c * 64 + f * 32, c * 64 + f * 32 + 32)
            rope[r, 0, :] = cos
            rope[r, 1, :] = -sin if f == 0 else sin
    ar = np.arange(128)
    oh = np.zeros((128, 4, 128), np.float32)
    sel = np.zeros((128, 64), np.float32)
    for q in range(4):
        for h in range(4):
            oh[32 * q + h, h, :] = 1.0
            sel[32 * q + h, 4 * q + h] = 1.0
    return {
        "c_rope": rope,
        "c_ident": np.eye(128, dtype=np.float32),
        "c_tri": (ar[:, None] <= ar[None, :]).astype(np.float32),
        "c_lstrict": (ar[:, None] < ar[None, :]).astype(np.float32),
        "c_maskneg": np.where(ar[:, None] <= ar[None, :], 0.0, -30000.0).astype(np.float32),
        "c_oh": oh,
        "c_sel": sel,
        "c_iota": np.ascontiguousarray(np.broadcast_to(np.arange(32, dtype=np.float32)[None, :], (128, 32))),
    }


def _in_maps(inputs):
    f32 = lambda a: np.ascontiguousarray(np.asarray(a, np.float32))
    x = f32(inputs["x"])
    wr = np.concatenate([f32(inputs["w_grp"][0]),
                         f32(inputs["w_erouter"][0]).transpose(1, 0, 2).reshape(D, 32)], axis=1)
    rb = np.concatenate([f32(inputs["b_grp"][0]), f32(inputs["b_erouter"][0]).reshape(32)])
    shared = {
        "w_in": f32(inputs["w_in"][0]),
        "w_out": f32(inputs["w_out"][0]),
        "w1": f32(inputs["w1"][0]),
        "w3": f32(inputs["w3"][0]),
        "w2": f32(inputs["w2"][0]),
        "lam_qk": f32(inputs["lam_qk"][0]),
        "subln_g": f32(inputs["subln_g"][0].reshape(128, 1)),
        "g_mix": f32(inputs["g_mix"][0].reshape(8, 128).T),
        "conv_w": f32(inputs["conv_w"][0].T.reshape(8, 128, 4).transpose(1, 0, 2)),
        "conv_b": f32(inputs["conv_b"][0].reshape(8, 128).T),
        "gate_b": f32(inputs["gate_b"][0].T),
        "mhg_bc": f32(np.broadcast_to(inputs["mhnorm_g"][0].reshape(1, 512), (128, 512))),
        "gffn_bc": f32(np.broadcast_to(inputs["g_ffn"][0].reshape(1, D), (128, D))),
        "gfin_bc": f32(np.broadcast_to(np.asarray(inputs["g_final"]).reshape(1, D), (128, D))),
        "gffn_col": f32(inputs["g_ffn"][0].reshape(8, 128).T),
        "wr": f32(wr.reshape(8, 128, 36).transpose(1, 0, 2)),
        "rbias": f32(np.broadcast_to(rb.reshape(1, 36), (128, 36))),
    }
    shared.update(_consts())
    maps = []
    for c in range(NCORES):
        m = {"x": np.ascontiguousarray(x[NSEQ * c:NSEQ * (c + 1)].reshape(TOK, D))}
        m.update(shared)
        maps.append(m)
    return maps


def kernel(**inputs):
    b = Builder()
    nc = b.build()
    maps = _in_maps(inputs)
    res = run_bass_kernel_spmd(nc, maps, core_ids=list(range(NCORES)))
    outs = [np.asarray(r["out"], dtype=np.float32).reshape(NSEQ, S, D) for r in res.results]
    return np.concatenate(outs, axis=0)
```
